# Optimizing a Trainium2 kernel written in Bass

```python
import jax, jax.numpy as jnp
from jax import lax
import numpy as np

D_MODEL = 1024
BATCH = 2
SEQ = 16384
DEPTH = 2

HEAD_DIM = 64
N_HEADS = D_MODEL // HEAD_DIM
H_FOX = N_HEADS // 2
H_NSA = N_HEADS - H_FOX
NSA_GROUPS = 2
H_MOBA = N_HEADS
D_FF = -(-(8 * D_MODEL) // (3 * 256)) * 256
ROPE_THETA = 10000.0
RMS_EPS = 1e-6
Q_BLOCK = 128
CMP_STRIDE = 16
CMP_LEN = 2 * CMP_STRIDE
CMP_HIDDEN = 4 * HEAD_DIM
SLC_BLOCK = 64
SLC_TOPN = 16
WINDOW = 512
MOBA_BLOCK = 256
MOBA_TOPK = 3
MOBA_Q_CHUNK = 32
NEG_INF = -1e30

EVEN_SPLITS = (3 * [H_FOX * HEAD_DIM] + [H_FOX] + [H_NSA * HEAD_DIM]
               + 6 * [NSA_GROUPS * HEAD_DIM] + [3 * H_NSA])
EVEN_IN = sum(EVEN_SPLITS)
EVEN_SPLIT_IDX = [int(c) for c in np.cumsum(EVEN_SPLITS)[:-1]]

kernel_name = 'hybrid_fox_nsa_moba_trunk'


def rmsnorm(x, g):
    xf = x.astype(jnp.float32)
    y = xf * lax.rsqrt(jnp.mean(xf * xf, axis=-1, keepdims=True) + RMS_EPS)
    return (y * g.astype(jnp.float32)).astype(x.dtype)


def to_heads(t, n):
    b, s, _ = t.shape
    return t.reshape(b, s, n, HEAD_DIM).transpose(0, 2, 1, 3)


def from_heads(t):
    b, h, s, d = t.shape
    return t.transpose(0, 2, 1, 3).reshape(b, s, h * d)


def rope_tables(s):
    inv = ROPE_THETA ** (-jnp.arange(0, HEAD_DIM, 2, dtype=jnp.float32) / HEAD_DIM)
    ang = jnp.arange(s, dtype=jnp.float32)[:, None] * inv[None, :]
    return jnp.cos(ang), jnp.sin(ang)


def apply_rope(t, cos, sin):
    tf = t.astype(jnp.float32)
    t1, t2 = tf[..., :HEAD_DIM // 2], tf[..., HEAD_DIM // 2:]
    return jnp.concatenate([t1 * cos - t2 * sin, t2 * cos + t1 * sin], axis=-1).astype(t.dtype)


def masked_softmax(s, mask):
    s = jnp.where(mask, s, NEG_INF)
    e = jnp.where(mask, jnp.exp(s - jnp.max(s, axis=-1, keepdims=True)), 0.0)
    return e / jnp.maximum(jnp.sum(e, axis=-1, keepdims=True), 1e-30)


def sweep(fn, s, block):
    out = lax.map(fn, jnp.arange(s // block))
    nb, b, h, q, d = out.shape
    return out.transpose(1, 2, 0, 3, 4).reshape(b, h, nb * q, d)


def fox_attention(q, k, v, log_f):
    b, h, s, d = q.shape
    c = jnp.cumsum(log_f, axis=-1)
    kpos = jnp.arange(s)
    scale = d ** -0.5

    def block(i):
        qs = i * Q_BLOCK
        qb = lax.dynamic_slice_in_dim(q, qs, Q_BLOCK, axis=2)
        cb = lax.dynamic_slice_in_dim(c, qs, Q_BLOCK, axis=2)
        qpos = qs + jnp.arange(Q_BLOCK)
        sc = (jnp.einsum('bhqd,bhkd->bhqk', qb, k, preferred_element_type=jnp.float32) * scale
              + cb[..., :, None] - c[..., None, :])
        p = masked_softmax(sc, kpos[None, :] <= qpos[:, None])
        return jnp.einsum('bhqk,bhkd->bhqd', p.astype(v.dtype), v)

    return sweep(block, s, Q_BLOCK)


def nsa_compress(t, pe, w1, w2):
    b, g, s, d = t.shape
    ch = t.reshape(b, g, s // CMP_STRIDE, CMP_STRIDE, d)
    blk = jnp.concatenate([ch[:, :, :-1], ch[:, :, 1:]], axis=3) + pe
    blk = blk.reshape(b, g, blk.shape[2], CMP_LEN * d)
    return jax.nn.gelu(blk @ w1) @ w2


def nsa_attention(q, kc, vc, ks, vs, kw, vw, gates):
    b, h, s, d = q.shape
    g = ks.shape[1]
    r = h // g
    nc = kc.shape[2]
    ns = s // SLC_BLOCK
    n_sel = min(SLC_TOPN, ns)
    ratio = SLC_BLOCK // CMP_STRIDE
    scale = d ** -0.5
    cmp_end = jnp.arange(nc) * CMP_STRIDE + CMP_LEN - 1
    ks_blk = ks.reshape(b, g, ns, SLC_BLOCK, d)
    vs_blk = vs.reshape(b, g, ns, SLC_BLOCK, d)
    wpad = ((0, 0), (0, 0), (WINDOW, 0), (0, 0))
    kw_pad, vw_pad = jnp.pad(kw, wpad), jnp.pad(vw, wpad)
    bi = jnp.arange(b)[:, None, None, None]
    gi = jnp.arange(g)[None, :, None, None]
    sblk = jnp.arange(ns)

    def block(i):
        qs = i * Q_BLOCK
        qpos = qs + jnp.arange(Q_BLOCK)
        qb = lax.dynamic_slice_in_dim(q, qs, Q_BLOCK, axis=2).reshape(b, g, r, Q_BLOCK, d)
        gb = lax.dynamic_slice_in_dim(gates, qs, Q_BLOCK, axis=2).reshape(b, g, r, Q_BLOCK, 3)
        sc = jnp.einsum('bgrqd,bgcd->bgrqc', qb, kc, preferred_element_type=jnp.float32) * scale
        pc = masked_softmax(sc, cmp_end[None, :] <= qpos[:, None])
        oc = jnp.einsum('bgrqc,bgcd->bgrqd', pc.astype(vc.dtype), vc)
        imp = jnp.pad(pc.sum(axis=2), ((0, 0), (0, 0), (0, 0), (1, 1)))
        imp = imp[..., :ratio * ns].reshape(b, g, Q_BLOCK, ns, ratio).sum(-1) + imp[..., ratio::ratio]
        qblk = qpos // SLC_BLOCK
        forced = ((sblk[None, :] == 0) | (sblk[None, :] == qblk[:, None])
                  | (sblk[None, :] == qblk[:, None] - 1))
        imp = jnp.where(forced, 1e9, jnp.where(sblk[None, :] > qblk[:, None], -1e9, imp))
        _, idx = lax.top_k(imp, n_sel)
        ksel = ks_blk[bi, gi, idx].reshape(b, g, Q_BLOCK, n_sel * SLC_BLOCK, d)
        vsel = vs_blk[bi, gi, idx].reshape(b, g, Q_BLOCK, n_sel * SLC_BLOCK, d)
        spos = (idx[..., None] * SLC_BLOCK + jnp.arange(SLC_BLOCK)).reshape(b, g, 1, Q_BLOCK, n_sel * SLC_BLOCK)
        ss = jnp.einsum('bgrqd,bgqkd->bgrqk', qb, ksel, preferred_element_type=jnp.float32) * scale
        psl = masked_softmax(ss, spos <= qpos[:, None])
        osl = jnp.einsum('bgrqk,bgqkd->bgrqd', psl.astype(vsel.dtype), vsel)
        kwin = lax.dynamic_slice_in_dim(kw_pad, qs, WINDOW + Q_BLOCK, axis=2)
        vwin = lax.dynamic_slice_in_dim(vw_pad, qs, WINDOW + Q_BLOCK, axis=2)
        wpos = qs - WINDOW + jnp.arange(WINDOW + Q_BLOCK)
        dlt = qpos[:, None] - wpos[None, :]
        sw = jnp.einsum('bgrqd,bgkd->bgrqk', qb, kwin, preferred_element_type=jnp.float32) * scale
        pw = masked_softmax(sw, (dlt >= 0) & (dlt < WINDOW) & (wpos[None, :] >= 0))
        ow = jnp.einsum('bgrqk,bgkd->bgrqd', pw.astype(vwin.dtype), vwin)
        o = gb[..., 0:1] * oc + gb[..., 1:2] * osl + gb[..., 2:3] * ow
        return o.reshape(b, h, Q_BLOCK, d).astype(q.dtype)

    return sweep(block, s, Q_BLOCK)


def moba_attention(q, k, v):
    b, h, s, d = q.shape
    s_pad = -(-s // MOBA_BLOCK) * MOBA_BLOCK
    pad = ((0, 0), (0, 0), (0, s_pad - s), (0, 0))
    kp, vp = jnp.pad(k, pad), jnp.pad(v, pad)
    nb = s_pad // MOBA_BLOCK
    n_top = min(MOBA_TOPK, nb)
    n_s = n_top * MOBA_BLOCK
    kb = kp.reshape(b, h, nb, MOBA_BLOCK, d)
    vb = vp.reshape(b, h, nb, MOBA_BLOCK, d)
    kbar = jnp.mean(kb.astype(jnp.float32), axis=3)
    bi = jnp.arange(b)[:, None, None, None]
    hi = jnp.arange(h)[None, :, None, None]
    blk_ids = jnp.arange(nb)
    scale = d ** -0.5

    def chunk(i):
        qs = i * MOBA_Q_CHUNK
        cur = qs // MOBA_BLOCK
        qpos = qs + jnp.arange(MOBA_Q_CHUNK)
        qb = lax.dynamic_slice_in_dim(q, qs, MOBA_Q_CHUNK, axis=2)
        gate = jnp.einsum('bhqd,bhnd->bhqn', qb.astype(jnp.float32), kbar)
        gate = jnp.where(blk_ids < cur, gate, NEG_INF)
        _, idx = lax.top_k(gate, n_top)
        valid = jnp.broadcast_to((idx < cur)[..., None], idx.shape + (MOBA_BLOCK,))
        valid = valid.reshape(b, h, MOBA_Q_CHUNK, n_s)
        ksel = kb[bi, hi, idx].reshape(b, h, MOBA_Q_CHUNK, n_s, d)
        vsel = vb[bi, hi, idx].reshape(b, h, MOBA_Q_CHUNK, n_s, d)
        kown = lax.dynamic_slice_in_dim(kp, cur * MOBA_BLOCK, MOBA_BLOCK, axis=2)
        vown = lax.dynamic_slice_in_dim(vp, cur * MOBA_BLOCK, MOBA_BLOCK, axis=2)
        own_pos = cur * MOBA_BLOCK + jnp.arange(MOBA_BLOCK)
        own_mask = jnp.broadcast_to(own_pos[None, :] <= qpos[:, None], (b, h, MOBA_Q_CHUNK, MOBA_BLOCK))
        s_sel = jnp.einsum('bhqd,bhqkd->bhqk', qb, ksel, preferred_element_type=jnp.float32)
        s_own = jnp.einsum('bhqd,bhkd->bhqk', qb, kown, preferred_element_type=jnp.float32)
        p = masked_softmax(jnp.concatenate([s_sel, s_own], axis=-1) * scale,
                           jnp.concatenate([valid, own_mask], axis=-1)).astype(v.dtype)
        return (jnp.einsum('bhqk,bhqkd->bhqd', p[..., :n_s], vsel)
                + jnp.einsum('bhqk,bhkd->bhqd', p[..., n_s:], vown))

    return sweep(chunk, s, MOBA_Q_CHUNK)


def even_mixer(h, w_in, b_f, pe_k, w1_k, w2_k, pe_v, w1_v, w2_v, w_out, cos, sin):
    b, s, _ = h.shape
    (fq, fk, fv, f_logit, nq, kc, vc, ks, vs, kw, vw, g_logit) = jnp.split(h @ w_in, EVEN_SPLIT_IDX, axis=-1)
    log_f = jax.nn.log_sigmoid(f_logit.astype(jnp.float32) + b_f.astype(jnp.float32)).transpose(0, 2, 1)
    o_fox = fox_attention(to_heads(fq, H_FOX), to_heads(fk, H_FOX), to_heads(fv, H_FOX), log_f)
    rot = lambda t, n: apply_rope(to_heads(t, n), cos, sin)
    gates = jax.nn.sigmoid(g_logit.astype(jnp.float32)).reshape(b, s, H_NSA, 3).transpose(0, 2, 1, 3)
    o_nsa = nsa_attention(rot(nq, H_NSA),
                          nsa_compress(rot(kc, NSA_GROUPS), pe_k, w1_k, w2_k),
                          nsa_compress(to_heads(vc, NSA_GROUPS), pe_v, w1_v, w2_v),
                          rot(ks, NSA_GROUPS), to_heads(vs, NSA_GROUPS),
                          rot(kw, NSA_GROUPS), to_heads(vw, NSA_GROUPS), gates)
    return jnp.concatenate([from_heads(o_fox), from_heads(o_nsa)], axis=-1) @ w_out


def odd_mixer(h, w_in, w_out, cos, sin):
    q, k, v = jnp.split(h @ w_in, 3, axis=-1)
    o = moba_attention(apply_rope(to_heads(q, H_MOBA), cos, sin),
                       apply_rope(to_heads(k, H_MOBA), cos, sin),
                       to_heads(v, H_MOBA))
    return from_heads(o) @ w_out


def swiglu(h, w_gate, w_up, w_down):
    return (jax.nn.silu(h @ w_gate) * (h @ w_up)) @ w_down


def setup_inputs(seed: int = 0) -> dict:
    key = jax.random.key(seed)
    ks = jax.random.split(key, 20)
    ne, no = (DEPTH + 1) // 2, DEPTH // 2
    f32 = jnp.float32

    def dense(k, shape, fan_in):
        return jax.random.normal(k, shape, f32) * fan_in ** -0.5

    def gain(k):
        return 1.0 + 0.05 * jax.random.normal(k, (DEPTH, D_MODEL), f32)

    cmp_in = CMP_LEN * HEAD_DIM
    return {
        'x': jax.random.normal(ks[0], (BATCH, SEQ, D_MODEL), f32),
        'ev_w_in': dense(ks[1], (ne, D_MODEL, EVEN_IN), D_MODEL),
        'ev_b_f': jax.random.uniform(ks[2], (ne, H_FOX), f32, 1.0, 6.0),
        'ev_cmp_pe_k': 0.5 * jax.random.normal(ks[3], (ne, CMP_LEN, HEAD_DIM), f32),
        'ev_cmp_w1_k': dense(ks[4], (ne, cmp_in, CMP_HIDDEN), cmp_in),
        'ev_cmp_w2_k': dense(ks[5], (ne, CMP_HIDDEN, HEAD_DIM), CMP_HIDDEN),
        'ev_cmp_pe_v': 0.5 * jax.random.normal(ks[6], (ne, CMP_LEN, HEAD_DIM), f32),
        'ev_cmp_w1_v': dense(ks[7], (ne, cmp_in, CMP_HIDDEN), cmp_in),
        'ev_cmp_w2_v': dense(ks[8], (ne, CMP_HIDDEN, HEAD_DIM), CMP_HIDDEN),
        'ev_w_out': dense(ks[9], (ne, (H_FOX + H_NSA) * HEAD_DIM, D_MODEL), (H_FOX + H_NSA) * HEAD_DIM),
        'od_w_in': dense(ks[10], (no, D_MODEL, 3 * H_MOBA * HEAD_DIM), D_MODEL),
        'od_w_out': dense(ks[11], (no, H_MOBA * HEAD_DIM, D_MODEL), H_MOBA * HEAD_DIM),
        'g_mix_pre': gain(ks[12]),
        'g_mix_post': gain(ks[13]),
        'g_ffn_pre': gain(ks[14]),
        'g_ffn_post': gain(ks[15]),
        'ffn_w_gate': dense(ks[16], (DEPTH, D_MODEL, D_FF), D_MODEL),
        'ffn_w_up': dense(ks[17], (DEPTH, D_MODEL, D_FF), D_MODEL),
        'ffn_w_down': dense(ks[18], (DEPTH, D_FF, D_MODEL), D_FF),
    }


def reference(x, ev_w_in, ev_b_f, ev_cmp_pe_k, ev_cmp_w1_k, ev_cmp_w2_k, ev_cmp_pe_v,
              ev_cmp_w1_v, ev_cmp_w2_v, ev_w_out, od_w_in, od_w_out, g_mix_pre, g_mix_post,
              g_ffn_pre, g_ffn_post, ffn_w_gate, ffn_w_up, ffn_w_down):
    cos, sin = rope_tables(x.shape[1])
    for layer in range(DEPTH):
        h = rmsnorm(x, g_mix_pre[layer])
        if layer % 2 == 0:
            e = layer // 2
            y = even_mixer(h, ev_w_in[e], ev_b_f[e], ev_cmp_pe_k[e], ev_cmp_w1_k[e], ev_cmp_w2_k[e],
                           ev_cmp_pe_v[e], ev_cmp_w1_v[e], ev_cmp_w2_v[e], ev_w_out[e], cos, sin)
        else:
            o = layer // 2
            y = odd_mixer(h, od_w_in[o], od_w_out[o], cos, sin)
        x = x + rmsnorm(y, g_mix_post[layer])
        h = rmsnorm(x, g_ffn_pre[layer])
        x = x + rmsnorm(swiglu(h, ffn_w_gate[layer], ffn_w_up[layer], ffn_w_down[layer]), g_ffn_post[layer])
    return x
```

```python
import numpy as np
import ml_dtypes
from contextlib import ExitStack
import concourse.bass as bass
import concourse.mybir as mybir
from concourse.bass_utils import run_bass_kernel_spmd

F32 = mybir.dt.float32
BF16 = mybir.dt.bfloat16
AF = mybir.ActivationFunctionType
ALU = mybir.AluOpType
AX = mybir.AxisListType
NPBF = ml_dtypes.bfloat16

D = 1024
HD = 64
DFF = 2816
EPS = 1e-6
NEG = -30000.0
NCORE = 8
EVEN_SPLITS = [512, 512, 512, 8, 512, 128, 128, 128, 128, 128, 128, 24]


class Cfg:
    def __init__(self, S):
        self.S = S
        self.TPC = 2 * S // NCORE
        self.NT = self.TPC // 512
        self.NG = S // 512
        self.NKT = S // 128
        self.LKT = self.TPC // 128
        assert self.NG % 8 == 0

    def owner(self, g):
        pair, pos = divmod(g, 8)
        return (pos, 2 * pair) if pos < 4 else (7 - pos, 2 * pair + 1)

    def gtile(self, r, j):
        pair, odd = divmod(j, 2)
        return pair * 8 + (r if odd == 0 else 7 - r)

    def kts_start(self, kts):
        r, rem = divmod(kts, self.LKT)
        j, q = divmod(rem, 4)
        return self.gtile(r, j) * 512 + q * 128

    def col_pos(self):
        pos = np.zeros(self.S, np.int64)
        for r in range(4):
            for j in range(self.NT):
                g = self.gtile(r, j)
                pos[r * self.TPC + j * 512: r * self.TPC + (j + 1) * 512] = g * 512 + np.arange(512)
        return pos


class Buf:
    __slots__ = ("ap", "w", "r", "dsem", "name")

    def __init__(self, ap, name=""):
        self.ap = ap
        self.w = {}
        self.r = {}
        self.dsem = None
        self.name = name


class Prog:
    def __init__(self):
        self.nc = bass.Bass("TRN2", target_bir_lowering=False)
        nc = self.nc
        self.es = ExitStack()
        self.E = dict(pe=nc.tensor, act=nc.scalar, dve=nc.vector, pool=nc.gpsimd, sp=nc.sync)
        self.sem = {}
        self.semh = {}
        for k in ("pe", "act", "dve", "pool"):
            s = self.es.enter_context(nc.semaphore("sem_" + k))
            self.sem[k] = s
            self.semh[id(s)] = s
        self.cnt = dict.fromkeys(self.sem, 0)
        self.waited = {k: {} for k in self.E}
        self.dsem_cnt = {}
        self.free_dsems = []
        self.phase_dsems = []
        self.nid = 0
        self.out_events = {}
        self.ninstr = 0
        self.pstack = None

    def _name(self, pfx):
        self.nid += 1
        return "%s_%d" % (pfx, self.nid)

    def begin_phase(self):
        self.pstack = ExitStack()
        self.phase_dsems = []

    def end_phase(self):
        self.barrier()
        self.pstack.close()
        self.pstack = None
        self.free_dsems.extend(self.phase_dsems)
        self.phase_dsems = []

    def barrier(self):
        allev = {id(self.sem[k]): self.cnt[k] for k in self.sem}
        allev.update(self.dsem_cnt)
        for e in self.E:
            self._wait(e, dict(allev))

    def sb(self, shape, dt, name="sb"):
        t = self.pstack.enter_context(self.nc.sbuf_tensor(self._name(name), list(shape), dt))
        return Buf(t, name)

    def ps(self, shape, dt=F32, name="ps"):
        t = self.pstack.enter_context(self.nc.psum_tensor(self._name(name), list(shape), dt))
        return Buf(t, name)

    def dram(self, name, shape, dt, kind):
        t = self.nc.dram_tensor(name, list(shape), dt, kind=kind)
        return Buf(t.ap(), name)

    def _wait(self, eng, deps):
        E = self.E[eng]
        wd = self.waited[eng]
        for sid, val in deps.items():
            if val <= 0:
                continue
            if sid in self.dsem_cnt:
                val = max(val, self.dsem_cnt[sid])
            if wd.get(sid, 0) < val:
                E.wait_ge(self.semh[sid], val)
                wd[sid] = val
                self.ninstr += 1

    @staticmethod
    def _merge(d, s):
        for k, v in s.items():
            if d.get(k, 0) < v:
                d[k] = v

    def op(self, eng, fn, reads=(), writes=(), inc=True):
        deps = {}
        for b in reads:
            self._merge(deps, b.w)
        for b in writes:
            self._merge(deps, b.w)
            self._merge(deps, b.r)
        if eng == "pe":
            deps.pop(id(self.sem["pe"]), None)
        self._wait(eng, deps)
        ins = fn(self.E[eng])
        self.ninstr += 1
        sid = id(self.sem[eng])
        if inc:
            self.cnt[eng] += 1
            ins.then_inc(self.sem[eng], 1)
            ev = self.cnt[eng]
        else:
            ev = self.cnt[eng] + 1
        for b in reads:
            if b.r.get(sid, 0) < ev:
                b.r[sid] = ev
        for b in writes:
            if b.w.get(sid, 0) < ev:
                b.w[sid] = ev
        return ins

    def dma(self, out_ap, in_ap, sbuf, reads=(), writes=(), q="sp", is_output=False):
        deps = {}
        for b in reads:
            self._merge(deps, b.w)
        for b in writes:
            self._merge(deps, b.w)
            self._merge(deps, b.r)
        self._wait(q, deps)
        if sbuf.dsem is None:
            if self.free_dsems:
                s = self.free_dsems.pop()
            else:
                s = self.es.enter_context(self.nc.semaphore(self._name("dsem")))
                self.semh[id(s)] = s
                self.dsem_cnt[id(s)] = 0
            sbuf.dsem = s
            self.phase_dsems.append(s)
        ins = self.E[q].dma_start(out=out_ap, in_=in_ap)
        self.ninstr += 1
        sid = id(sbuf.dsem)
        self.dsem_cnt[sid] += 16
        ev = self.dsem_cnt[sid]
        ins.then_inc(sbuf.dsem, 16)
        for b in reads:
            b.r[sid] = ev
        for b in writes:
            b.w[sid] = ev
        if is_output:
            self.out_events[sid] = ev
        return ins

    def finish(self):
        self._wait("sp", dict(self.out_events))
        self.barrier()


class Ctx:
    pass


def load_consts(p, cst):
    c = Ctx()
    c.ident = p.sb([128, 128], BF16, "ident")
    p.dma(c.ident.ap[:], cst["ident"].ap[:, :], c.ident, writes=[c.ident])
    c.ones = p.sb([128, 128], BF16, "ones")
    p.op("pool", lambda e: e.memset(c.ones.ap[:], 1.0), writes=[c.ones])
    c.ones32 = p.sb([128, 128], F32, "ones32")
    p.op("pool", lambda e: e.memset(c.ones32.ap[:], 1.0), writes=[c.ones32])
    return c


def load_weight(p, wsb, w_dram, K, N, stage):
    i = 0
    for kc in range(K // 128):
        for n0 in range(0, N, 2048):
            n = min(2048, N - n0)
            st = stage[i % len(stage)]
            i += 1
            p.dma(st.ap[:, 0:n], w_dram.ap[kc * 128:(kc + 1) * 128, n0:n0 + n], st, writes=[st])
            p.op("pool", lambda e, st=st, kc=kc, n0=n0, n=n: e.tensor_copy(out=wsb.ap[:, kc, n0:n0 + n], in_=st.ap[:, 0:n]),
                 reads=[st], writes=[wsb])


def rms_rstd(p, c, chunks_ap, chunk_bufs, N, ps_ss, sqb, tmp, rstd):
    nch = len(chunks_ap)
    for k in range(nch):
        sq = sqb[k % len(sqb)]
        p.op("pool", lambda e, k=k, sq=sq: e.tensor_tensor(out=sq.ap[:, 0:N], in0=chunks_ap[k], in1=chunks_ap[k], op=ALU.mult),
             reads=[chunk_bufs[k]], writes=[sq])
        p.op("pe", lambda e, k=k, sq=sq: e.matmul(ps_ss.ap[:, 0:N], lhsT=c.ones.ap[:], rhs=sq.ap[:, 0:N], start=(k == 0), stop=(k == nch - 1)),
             reads=[c.ones, sq], writes=[ps_ss], inc=True)
    p.op("act", lambda e: e.activation(out=tmp.ap[:, 0:N], in_=ps_ss.ap[:, 0:N], func=AF.Sqrt, scale=1.0 / (128 * nch), bias=c.epsb.ap[:, 0:1]),
         reads=[ps_ss, c.epsb], writes=[tmp])
    p.op("dve", lambda e: e.reciprocal(out=rstd.ap[:, 0:N], in_=tmp.ap[:, 0:N]), reads=[tmp], writes=[rstd])


def make_eps(p, c):
    c.epsb = p.sb([128, 1], F32, "epsb")
    p.op("pool", lambda e: e.memset(c.epsb.ap[:], EPS), writes=[c.epsb])


def phase_C(p, cfg, cst, oT, xT_in, xT_out, w_out, w_gate, w_up, w_down, gains, hT_d, mT_d, x1T_d, final):
    TPC = cfg.TPC
    NTT = TPC // 512
    p.begin_phase()
    c = load_consts(p, cst)
    make_eps(p, c)
    g = p.sb([128, 4, 8], F32, "gains")
    p.dma(g.ap[:], gains.ap[:, :, :], g, writes=[g])
    stage = [p.sb([128, 2048], F32, "stage") for _ in range(2)]
    wo = p.sb([128, 8, 1024], BF16, "wo")
    load_weight(p, wo, w_out, 1024, 1024, stage)
    banks = [p.ps([128, 512], F32, "bank") for _ in range(6)]
    ps_ss = p.ps([128, 512], F32, "ss")
    oTs = [p.sb([128, 8, 512], BF16, "oTs") for _ in range(2)]
    xs = [p.sb([128, 8, 512], F32, "xs") for _ in range(2)]
    ys = [[p.sb([128, 512], F32, "ys") for _ in range(8)] for _ in range(1)]
    sqb = [p.sb([128, 512], BF16, "sq") for _ in range(2)]
    tmp = p.sb([128, 512], F32, "tmp")
    rstd = p.sb([128, 512], F32, "rstd")
    rstd2 = p.sb([128, 512], F32, "rstd2")
    tt_ = [p.sb([128, 512], F32, "tt") for _ in range(2)]
    x1 = [p.sb([128, 8, 512], F32, "x1") for _ in range(2)]
    hb = [p.sb([128, 8, 512], BF16, "hb") for _ in range(2)]
    for t in range(NTT):
        cs = slice(t * 512, (t + 1) * 512)
        o_s, x_s, x1_s, h_s = oTs[t % 2], xs[t % 2], x1[t % 2], hb[t % 2]
        p.dma(o_s.ap[:], oT.ap[:, cs].rearrange("(k p) n -> p k n", p=128), o_s, reads=[oT], writes=[o_s])
        p.dma(x_s.ap[:], xT_in.ap[:, cs].rearrange("(k p) n -> p k n", p=128), x_s, reads=[xT_in], writes=[x_s])
        for oc in range(8):
            bk = banks[oc % 6]
            for kc in range(8):
                p.op("pe", lambda e, oc=oc, kc=kc, bk=bk: e.matmul(bk.ap[:], lhsT=wo.ap[:, kc, oc * 128:(oc + 1) * 128], rhs=o_s.ap[:, kc, :],
                                                                   start=(kc == 0), stop=(kc == 7)),
                     reads=[wo, o_s], writes=[bk], inc=(kc == 7))
            p.op("act", lambda e, oc=oc, bk=bk: e.activation(out=ys[0][oc].ap[:], in_=bk.ap[:], func=AF.Copy), reads=[bk], writes=[ys[0][oc]])
        rms_rstd(p, c, [ys[0][k].ap[:] for k in range(8)], ys[0], 512, ps_ss, sqb, tmp, rstd)
        for oc in range(8):
            tb = tt_[oc % 2]
            p.op("dve", lambda e, oc=oc, tb=tb: e.tensor_tensor(out=tb.ap[:], in0=ys[0][oc].ap[:], in1=rstd.ap[:], op=ALU.mult),
                 reads=[ys[0][oc], rstd], writes=[tb])
            p.op("dve", lambda e, oc=oc, tb=tb: e.scalar_tensor_tensor(out=x1_s.ap[:, oc, :], in0=tb.ap[:], scalar=g.ap[:, 0, oc:oc + 1],
                                                                       in1=x_s.ap[:, oc, :], op0=ALU.mult, op1=ALU.add),
                 reads=[tb, g, x_s], writes=[x1_s])
        p.dma(x1T_d.ap[:, cs].rearrange("(k p) n -> p k n", p=128), x1_s.ap[:], x1_s, reads=[x1_s], writes=[x1T_d])
        rms_rstd(p, c, [x1_s.ap[:, k, :] for k in range(8)], [x1_s] * 8, 512, ps_ss, sqb, tmp, rstd2)
        for oc in range(8):
            p.op("dve", lambda e, oc=oc: e.scalar_tensor_tensor(out=h_s.ap[:, oc, :], in0=x1_s.ap[:, oc, :], scalar=g.ap[:, 1, oc:oc + 1],
                                                                in1=rstd2.ap[:], op0=ALU.mult, op1=ALU.mult),
                 reads=[x1_s, g, rstd2], writes=[h_s])
        p.dma(hT_d.ap[:, cs].rearrange("(k p) n -> p k n", p=128), h_s.ap[:], h_s, reads=[h_s], writes=[hT_d])
    p.end_phase()
    p.begin_phase()
    stage = [p.sb([128, 2048], F32, "stage") for _ in range(2)]
    wg = p.sb([128, 8, DFF], BF16, "wg")
    wu = p.sb([128, 8, DFF], BF16, "wu")
    load_weight(p, wg, w_gate, 1024, DFF, stage)
    load_weight(p, wu, w_up, 1024, DFF, stage)
    banks = [p.ps([128, 512], F32, "bank") for _ in range(8)]
    hs = [p.sb([128, 8, 512], BF16, "hs") for _ in range(2)]
    ms = [p.sb([128, 11, 512], BF16, "ms") for _ in range(2)]
    sil = [p.sb([128, 512], F32, "sil") for _ in range(3)]
    NFC = DFF // 128
    mi = 0
    for t in range(NTT):
        cs = slice(t * 512, (t + 1) * 512)
        h_s = hs[t % 2]
        p.dma(h_s.ap[:], hT_d.ap[:, cs].rearrange("(k p) n -> p k n", p=128), h_s, reads=[hT_d], writes=[h_s])
        for half in range(2):
            m_s = ms[mi % 2]
            mi += 1
            for f in range(11):
                fc = half * 11 + f
                bg, bu = banks[(2 * fc) % 8], banks[(2 * fc + 1) % 8]
                for kc in range(8):
                    p.op("pe", lambda e, fc=fc, kc=kc, bg=bg: e.matmul(bg.ap[:], lhsT=wg.ap[:, kc, fc * 128:(fc + 1) * 128], rhs=h_s.ap[:, kc, :],
                                                                       start=(kc == 0), stop=(kc == 7)), reads=[wg, h_s], writes=[bg], inc=(kc == 7))
                for kc in range(8):
                    p.op("pe", lambda e, fc=fc, kc=kc, bu=bu: e.matmul(bu.ap[:], lhsT=wu.ap[:, kc, fc * 128:(fc + 1) * 128], rhs=h_s.ap[:, kc, :],
                                                                       start=(kc == 0), stop=(kc == 7)), reads=[wu, h_s], writes=[bu], inc=(kc == 7))
                sb_ = sil[fc % 3]
                p.op("act", lambda e, bg=bg, sb_=sb_: e.activation(out=sb_.ap[:], in_=bg.ap[:], func=AF.Silu), reads=[bg], writes=[sb_])
                p.op("dve", lambda e, bu=bu, sb_=sb_, f=f, m_s=m_s: e.tensor_tensor(out=m_s.ap[:, f, :], in0=bu.ap[:], in1=sb_.ap[:], op=ALU.mult),
                     reads=[bu, sb_], writes=[m_s])
            p.dma(mT_d.ap[half * 1408:(half + 1) * 1408, cs].rearrange("(k p) n -> p k n", p=128), m_s.ap[:], m_s, reads=[m_s], writes=[mT_d])
    p.end_phase()
    p.begin_phase()
    c = load_consts(p, cst)
    make_eps(p, c)
    g = p.sb([128, 4, 8], F32, "gains")
    p.dma(g.ap[:], gains.ap[:, :, :], g, writes=[g])
    stage = [p.sb([128, 2048], F32, "stage") for _ in range(2)]
    wd = p.sb([128, NFC, 1024], BF16, "wd")
    load_weight(p, wd, w_down, DFF, 1024, stage)
    banks = [p.ps([128, 512], F32, "bank") for _ in range(6)]
    ps_ss = p.ps([128, 512], F32, "ss")
    ms = [p.sb([128, NFC, 512], BF16, "ms") for _ in range(1)]
    xs = [p.sb([128, 8, 512], F32, "xs") for _ in range(2)]
    xo = [p.sb([128, 8, 512], F32, "xo") for _ in range(2)]
    ys = [p.sb([128, 512], F32, "ys") for _ in range(8)]
    sqb = [p.sb([128, 512], BF16, "sq") for _ in range(2)]
    tmp = p.sb([128, 512], F32, "tmp")
    rstd = p.sb([128, 512], F32, "rstd")
    tt_ = [p.sb([128, 512], F32, "tt") for _ in range(2)]
    for t in range(NTT):
        cs = slice(t * 512, (t + 1) * 512)
        m_s, x_s, xo_s = ms[0], xs[t % 2], xo[t % 2]
        p.dma(m_s.ap[:], mT_d.ap[:, cs].rearrange("(k p) n -> p k n", p=128), m_s, reads=[mT_d], writes=[m_s])
        p.dma(x_s.ap[:], x1T_d.ap[:, cs].rearrange("(k p) n -> p k n", p=128), x_s, reads=[x1T_d], writes=[x_s])
        for oc in range(8):
            bk = banks[oc % 6]
            for fc in range(NFC):
                p.op("pe", lambda e, oc=oc, fc=fc, bk=bk: e.matmul(bk.ap[:], lhsT=wd.ap[:, fc, oc * 128:(oc + 1) * 128], rhs=m_s.ap[:, fc, :],
                                                                   start=(fc == 0), stop=(fc == NFC - 1)), reads=[wd, m_s], writes=[bk], inc=(fc == NFC - 1))
            p.op("act", lambda e, oc=oc, bk=bk: e.activation(out=ys[oc].ap[:], in_=bk.ap[:], func=AF.Copy), reads=[bk], writes=[ys[oc]])
        rms_rstd(p, c, [ys[k].ap[:] for k in range(8)], ys, 512, ps_ss, sqb, tmp, rstd)
        for oc in range(8):
            tb = tt_[oc % 2]
            p.op("dve", lambda e, oc=oc, tb=tb: e.tensor_tensor(out=tb.ap[:], in0=ys[oc].ap[:], in1=rstd.ap[:], op=ALU.mult),
                 reads=[ys[oc], rstd], writes=[tb])
            p.op("dve", lambda e, oc=oc, tb=tb: e.scalar_tensor_tensor(out=xo_s.ap[:, oc, :], in0=tb.ap[:], scalar=g.ap[:, 2, oc:oc + 1],
                                                                       in1=x_s.ap[:, oc, :], op0=ALU.mult, op1=ALU.add),
                 reads=[tb, g, x_s], writes=[xo_s])
        p.dma(xT_out.ap[:, cs].rearrange("(k p) n -> p k n", p=128), xo_s.ap[:], xo_s, reads=[xo_s], writes=[xT_out], is_output=final)
    p.end_phase()


def phase_A(p, cfg, cst, even, xT, W, ncols, gains, cosT, sinT, bf_d, outs):
    TPC = cfg.TPC
    NTT = TPC // 512
    p.begin_phase()
    c = load_consts(p, cst)
    make_eps(p, c)
    g = p.sb([128, 4, 8], F32, "gains")
    p.dma(g.ap[:], gains.ap[:, :, :], g, writes=[g])
    stage = [p.sb([128, 2048], F32, "stage") for _ in range(2)]
    w = p.sb([128, 8, ncols], BF16, "w")
    load_weight(p, w, W, 1024, ncols, stage)
    banks = [p.ps([128, 512], F32, "bank") for _ in range(7)]
    ps_ss = p.ps([128, 512], F32, "ss")
    xs = [p.sb([128, 8, 512], F32, "xs") for _ in range(2)]
    hb = [p.sb([128, 8, 512], BF16, "hb") for _ in range(2)]
    sqb = [p.sb([128, 512], BF16, "sq") for _ in range(2)]
    tmp = p.sb([128, 512], F32, "tmp")
    rstd = p.sb([128, 512], F32, "rstd")
    cs_t = [p.sb([128, 2, 512], F32, "cossin") for _ in range(2)]
    ob = [p.sb([128, 4, 512], BF16, "ob") for _ in range(3)]
    r1 = [p.sb([128, 512], F32, "r1") for _ in range(2)]
    r2 = [p.sb([128, 512], F32, "r2") for _ in range(2)]
    nvh = 16 if not even else 12
    vst = [p.sb([128, nvh, 4, 65], BF16, "vst") for _ in range(2)]
    for v in vst:
        p.op("pool", lambda e, v=v: e.memset(v.ap[:], 1.0), writes=[v])
    if even:
        bfs = p.sb([8, 1], F32, "bfs")
        p.dma(bfs.ap[:], bf_d.ap[:, :], bfs, writes=[bfs])
        nbf = p.sb([8, 1], F32, "nbf")
        p.op("pool", lambda e: e.tensor_scalar(out=nbf.ap[:], in0=bfs.ap[:], scalar1=-1.0, scalar2=None, op0=ALU.mult), reads=[bfs], writes=[nbf])
        e1 = [p.sb([8, 512], F32, "e1") for _ in range(2)]
        lf = [p.sb([8, 512], F32, "lf") for _ in range(2)]
        gt = [p.sb([64, 512], F32, "gt") for _ in range(2)]
    state = dict(bk=0, ob=0, r=0)

    def nbank():
        b = banks[state["bk"] % 7]
        state["bk"] += 1
        return b

    def proj_fm(h_s, col0, M=128):
        bk = nbank()
        for kc in range(8):
            p.op("pe", lambda e, kc=kc, bk=bk: e.matmul(bk.ap[0:M, :], lhsT=w.ap[:, kc, col0:col0 + M], rhs=h_s.ap[:, kc, :],
                                                        start=(kc == 0), stop=(kc == 7)), reads=[w, h_s], writes=[bk], inc=(kc == 7))
        return bk

    for t in range(NTT):
        cs = slice(t * 512, (t + 1) * 512)
        x_s, h_s, cs_s = xs[t % 2], hb[t % 2], cs_t[t % 2]
        p.dma(x_s.ap[:], xT.ap[:, cs].rearrange("(k p) n -> p k n", p=128), x_s, reads=[xT], writes=[x_s])
        p.dma(cs_s.ap[:, 0, :], cosT.ap[:, cs], cs_s, writes=[cs_s])
        p.dma(cs_s.ap[:, 1, :], sinT.ap[:, cs], cs_s, writes=[cs_s])
        rms_rstd(p, c, [x_s.ap[:, k, :] for k in range(8)], [x_s] * 8, 512, ps_ss, sqb, tmp, rstd)
        for oc in range(8):
            p.op("dve", lambda e, oc=oc: e.scalar_tensor_tensor(out=h_s.ap[:, oc, :], in0=x_s.ap[:, oc, :], scalar=g.ap[:, 3, oc:oc + 1],
                                                                in1=rstd.ap[:], op0=ALU.mult, op1=ALU.mult),
                 reads=[x_s, g, rstd], writes=[h_s])

        def fm_group(col0, nch, dst, row0, rot0=None):
            for c0 in range(0, nch, 4):
                n = min(4, nch - c0)
                o_s = ob[state["ob"] % 3]
                state["ob"] += 1
                for i in range(n):
                    ch = c0 + i
                    bk = proj_fm(h_s, col0 + ch * 128)
                    if rot0 is None:
                        p.op("act", lambda e, bk=bk, i=i, o_s=o_s: e.activation(out=o_s.ap[:, i, :], in_=bk.ap[:], func=AF.Copy), reads=[bk], writes=[o_s])
                    else:
                        bk2 = proj_fm(h_s, rot0 + ch * 128)
                        a1, a2 = r1[state["r"] % 2], r2[state["r"] % 2]
                        state["r"] += 1
                        p.op("dve", lambda e, bk=bk, a1=a1: e.tensor_tensor(out=a1.ap[:], in0=bk.ap[:], in1=cs_s.ap[:, 0, :], op=ALU.mult), reads=[bk, cs_s], writes=[a1])
                        p.op("dve", lambda e, bk2=bk2, a2=a2: e.tensor_tensor(out=a2.ap[:], in0=bk2.ap[:], in1=cs_s.ap[:, 1, :], op=ALU.mult), reads=[bk2, cs_s], writes=[a2])
                        p.op("pool", lambda e, a1=a1, a2=a2, i=i, o_s=o_s: e.tensor_tensor(out=o_s.ap[:, i, :], in0=a1.ap[:], in1=a2.ap[:], op=ALU.add), reads=[a1, a2], writes=[o_s])
                r0 = row0 + c0 * 128
                p.dma(dst.ap[r0:r0 + n * 128, cs].rearrange("(k p) n -> p k n", p=128), o_s.ap[:, 0:n, :], o_s, reads=[o_s], writes=[dst])

        def tok_group(col0, groups, v_s):
            for sub in range(4):
                for (cr, ncol, h0) in groups:
                    bk = nbank()
                    for kc in range(8):
                        p.op("pe", lambda e, kc=kc, bk=bk, cr=cr, ncol=ncol, sub=sub: e.matmul(
                            bk.ap[:, 0:ncol], lhsT=h_s.ap[:, kc, sub * 128:(sub + 1) * 128], rhs=w.ap[:, kc, col0 + cr:col0 + cr + ncol],
                            start=(kc == 0), stop=(kc == 7)), reads=[w, h_s], writes=[bk], inc=(kc == 7))
                    nh = ncol // 64
                    p.op("act", lambda e, bk=bk, ncol=ncol, h0=h0, nh=nh, sub=sub: e.activation(
                        out=v_s.ap[:, h0:h0 + nh, sub, 0:64], in_=bk.ap[:, 0:ncol].rearrange("p (h d) -> p h d", d=64), func=AF.Copy),
                        reads=[bk], writes=[v_s])

        v_s = vst[t % 2]
        if not even:
            fm_group(0, 8, outs["qT"], 0, rot0=1024)
            fm_group(2048, 8, outs["kT"], 0, rot0=3072)
            tok_group(4096, [(0, 512, 0), (512, 512, 8)], v_s)
            p.dma(outs["vp"].ap[:, :, t * 4:(t + 1) * 4, :].rearrange("h p k d -> p h k d"), v_s.ap[:], v_s, reads=[v_s], writes=[outs["vp"]])
        else:
            fm_group(0, 4, outs["fqT"], 0)
            fm_group(512, 4, outs["fkT"], 0)
            fm_group(1024, 4, outs["nqT"], 0, rot0=1536)
            fm_group(2048, 3, outs["nkT"], 0, rot0=2432)
            fm_group(2816, 1, outs["vcT"], 0)
            bk = proj_fm(h_s, 2944, M=64)
            e_s, l_s, g_s = e1[t % 2], lf[t % 2], gt[t % 2]
            p.op("act", lambda e, bk=bk, e_s=e_s: e.activation(out=e_s.ap[:], in_=bk.ap[0:8, :], func=AF.Exp, scale=-1.0, bias=nbf.ap[:, 0:1]), reads=[bk, nbf], writes=[e_s])
            p.op("act", lambda e, e_s=e_s, l_s=l_s: e.activation(out=l_s.ap[:], in_=e_s.ap[:], func=AF.Ln, bias=c.ones32.ap[0:8, 0:1]), reads=[e_s, c.ones32], writes=[l_s])
            p.op("pool", lambda e, l_s=l_s: e.tensor_scalar(out=l_s.ap[:], in0=l_s.ap[:], scalar1=-1.0, scalar2=None, op0=ALU.mult), reads=[l_s], writes=[l_s])
            p.dma(outs["logfT"].ap[:, cs], l_s.ap[:], l_s, reads=[l_s], writes=[outs["logfT"]])
            p.op("act", lambda e, bk=bk, g_s=g_s: e.activation(out=g_s.ap[32:56, :], in_=bk.ap[32:56, :], func=AF.Sigmoid), reads=[bk], writes=[g_s])
            p.dma(outs["gatesT"].ap[:, cs], g_s.ap[32:56, :], g_s, reads=[g_s], writes=[outs["gatesT"]])
            tok_group(3072, [(0, 512, 0), (512, 256, 8)], v_s)
            p.dma(outs["vp"].ap[:, :, t * 4:(t + 1) * 4, :].rearrange("h p k d -> p h k d"), v_s.ap[:], v_s, reads=[v_s], writes=[outs["vp"]])
    p.end_phase()


class Attn:
    def __init__(self, p, c, cst, n_s=3):
        self.c = c
        self.S = [p.ps([128, 512], F32, "S") for _ in range(n_s)]
        self.P = [p.sb([128, 512], BF16, "P") for _ in range(3)]
        self.O = [p.ps([128, 512], F32, "O") for _ in range(2)]
        self.bc = p.ps([128, 512], F32, "bc")
        self.mpat = p.sb([128, 13, 512], BF16, "mpat")
        p.dma(self.mpat.ap[:], cst["mpat"].ap[:, :, :], self.mpat, writes=[self.mpat])
        self.srow = [p.sb([65, 512], F32, "srow") for _ in range(2)]
        self.bcs = [p.sb([64, 512], F32, "bcs") for _ in range(2)]
        self.scnt = 0
        self.ocnt = 0
        self.ncnt = 0


def attention(p, A, Qfn, Qbufs, Kfn, Kbuf, Vfn, Vbuf, tiles, scale):
    c = A.c
    O = A.O[A.ocnt % 2]
    A.ocnt += 1
    n = len(tiles)
    slots = []
    assert tiles[0][2] == 0 and tiles[0][3] == 512
    for i in range(n + 2):
        if i < n:
            kt, pat, q0, q1, qsel = tiles[i]
            Sb = A.S[A.scnt % len(A.S)]
            Pb = A.P[A.scnt % 3]
            A.scnt += 1
            slots.append((Sb, Pb))
            qap = Qfn(qsel)
            p.op("pe", lambda e, Sb=Sb, kt=kt, qap=qap, q0=q0, q1=q1, pat=pat: e.matmul(Sb.ap[:, q0:q1], lhsT=Kfn(kt), rhs=qap[:, q0:q1], start=True, stop=(pat is None)),
                 reads=[Kbuf] + list(Qbufs), writes=[Sb], inc=(pat is None))
            if pat is not None:
                p.op("pe", lambda e, Sb=Sb, q0=q0, q1=q1, pat=pat: e.matmul(Sb.ap[:, q0:q1], lhsT=c.ident.ap[:], rhs=A.mpat.ap[:, pat, q0:q1], start=False, stop=True),
                     reads=[A.mpat, c.ident], writes=[Sb])
            p.op("act", lambda e, Sb=Sb, Pb=Pb, q0=q0, q1=q1: e.activation(out=Pb.ap[:, q0:q1], in_=Sb.ap[:, q0:q1], func=AF.Exp, scale=scale),
                 reads=[Sb], writes=[Pb])
        if i >= 2:
            j = i - 2
            kt, pat, q0, q1, qsel = tiles[j]
            Sb, Pb = slots[j]
            p.op("pe", lambda e, Pb=Pb, kt=kt, q0=q0, q1=q1, j=j: e.matmul(O.ap[0:65, q0:q1], lhsT=Vfn(kt), rhs=Pb.ap[:, q0:q1], start=(j == 0), stop=(j == n - 1)),
                 reads=[Vbuf, Pb], writes=[O])
    return O


def normalize(p, A, O, gate_ap=None, gate_buf=None):
    c = A.c
    sr = A.srow[A.ncnt % 2]
    bcs = A.bcs[A.ncnt % 2]
    A.ncnt += 1
    p.op("act", lambda e: e.activation(out=sr.ap[64:65, :], in_=O.ap[64:65, :], func=AF.Copy), reads=[O], writes=[sr])
    p.op("dve", lambda e: e.tensor_scalar(out=sr.ap[64:65, :], in0=sr.ap[64:65, :], scalar1=1e-30, scalar2=None, op0=ALU.max), reads=[sr], writes=[sr])
    p.op("dve", lambda e: e.reciprocal(out=sr.ap[64:65, :], in_=sr.ap[64:65, :]), reads=[sr], writes=[sr])
    if gate_ap is not None:
        p.op("dve", lambda e: e.tensor_tensor(out=sr.ap[64:65, :], in0=sr.ap[64:65, :], in1=gate_ap, op=ALU.mult), reads=[sr, gate_buf], writes=[sr])
    p.op("pe", lambda e: e.matmul(A.bc.ap[0:64, :], lhsT=c.ones32.ap[64:65, 0:64], rhs=sr.ap[64:65, :], start=True, stop=True),
         reads=[sr, c.ones32], writes=[A.bc])
    p.op("act", lambda e: e.activation(out=bcs.ap[:], in_=A.bc.ap[0:64, :], func=AF.Copy), reads=[A.bc], writes=[bcs])
    return bcs


def causal_tiles(g, qsel_fn=lambda kt: 0):
    tl = [(kt, None, 0, 512, qsel_fn(kt)) for kt in range(4 * g)]
    tl += [(4 * g + d, d, 128 * d, 512, qsel_fn(4 * g + d)) for d in range(4)]
    return tl


def phase_B_moba(p, cfg, cst, qT, kT, vp, oT):
    S = cfg.S
    NKT = cfg.NKT
    NG = cfg.NG
    NB = S // 256
    p.begin_phase()
    c = load_consts(p, cst)
    A = Attn(p, c, cst, n_s=3)
    gbank = p.ps([128, 512], F32, "gbank")
    tbank = p.ps([128, 1024], BF16, "tbank")
    Kp = [p.sb([128, S], BF16, "Kp") for _ in range(2)]
    Vs = [p.sb([128, NKT, 65], BF16, "Vs") for _ in range(2)]
    Qp = [p.sb([128, 512], BF16, "Qp") for _ in range(2)]
    stat = p.sb([128, 128], F32, "stat")
    p.dma(stat.ap[:], cst["moba_stat"].ap[:, :], stat, writes=[stat])
    for k in Kp:
        p.dma(k.ap[64:128, :], cst["moba_oh"].ap[:, :], k, writes=[k])
    kbar32 = p.sb([64, NB], F32, "kbar32")
    kbar = [p.sb([64, NB], BF16, "kbar") for _ in range(2)]
    vv = [p.sb([128, 4, NB], F32, "vv") for _ in range(2)]
    m8 = [p.sb([128, 4, 8], F32, "m8") for _ in range(2)]
    thr = [p.sb([128, 4], F32, "thr") for _ in range(2)]
    mt = [p.sb([128, 4, 128], BF16, "mt") for _ in range(2)]
    for m in mt:
        p.op("pool", lambda e, m=m: e.memset(m.ap[:], 0.0), writes=[m])
    osb = [p.sb([64, 512], BF16, "osb") for _ in range(2)]
    for h in range(4):
        K_s, V_s, kb = Kp[h % 2], Vs[h % 2], kbar[h % 2]
        p.dma(K_s.ap[0:64, :], kT.ap[h * 64:(h + 1) * 64, :], K_s, reads=[kT], writes=[K_s])
        p.dma(V_s.ap[:], vp.ap[h, :, :, :], V_s, reads=[vp], writes=[V_s])
        p.op("dve", lambda e, K_s=K_s: e.tensor_reduce(out=kbar32.ap[:], in_=K_s.ap[0:64, :].rearrange("p (b k) -> p b k", k=256), axis=AX.X, op=ALU.add),
             reads=[K_s], writes=[kbar32])
        p.op("dve", lambda e, kb=kb: e.tensor_scalar(out=kb.ap[:], in0=kbar32.ap[:], scalar1=1.0 / 256, scalar2=None, op0=ALU.mult), reads=[kbar32], writes=[kb])

        def build1(g, h=h, kb=kb):
            i = g % 2
            Q_s, v_s, m8_s, th_s, mt_s = Qp[i], vv[i], m8[i], thr[i], mt[i]
            cs = slice(g * 512, (g + 1) * 512)
            p.dma(Q_s.ap[0:64, :], qT.ap[h * 64:(h + 1) * 64, cs], Q_s, reads=[qT], writes=[Q_s])
            for sub in range(4):
                p.op("pe", lambda e, sub=sub: e.matmul(gbank.ap[:, sub * 64:sub * 64 + NB], lhsT=Q_s.ap[0:64, sub * 128:(sub + 1) * 128], rhs=kb.ap[:],
                                                       start=True, stop=True), reads=[Q_s, kb], writes=[gbank], inc=(sub == 3))
            for sub in range(4):
                cur = 2 * g + sub // 2
                p.op("dve", lambda e, sub=sub, cur=cur: e.tensor_tensor(out=v_s.ap[:, sub, :], in0=gbank.ap[:, sub * 64:sub * 64 + NB], in1=stat.ap[:, 64 - cur:64 - cur + NB], op=ALU.add),
                     reads=[gbank, stat], writes=[v_s])
            for sub in range(4):
                p.op("dve", lambda e, sub=sub: e.max(out=m8_s.ap[:, sub, :], in_=v_s.ap[:, sub, :]), reads=[v_s], writes=[m8_s])
            p.op("dve", lambda e: e.tensor_scalar(out=th_s.ap[:], in0=m8_s.ap[:, :, 3], scalar1=-1e29, scalar2=None, op0=ALU.max), reads=[m8_s], writes=[th_s])
            for sub in range(4):
                p.op("dve", lambda e, sub=sub: e.tensor_scalar(out=mt_s.ap[:, sub, 64:64 + NB], in0=v_s.ap[:, sub, :], scalar1=th_s.ap[:, sub:sub + 1], scalar2=-1.0,
                                                               op0=ALU.is_ge, op1=ALU.add), reads=[v_s, th_s], writes=[mt_s])

        def build2(g):
            i = g % 2
            Q_s, mt_s = Qp[i], mt[i]
            for sub in range(4):
                p.op("pe", lambda e, sub=sub: e.transpose(out=tbank.ap[:, sub * 128:(sub + 1) * 128], in_=mt_s.ap[:, sub, :], identity=c.ident.ap[:]),
                     reads=[mt_s, c.ident], writes=[tbank], inc=(sub == 3))
            p.op("dve", lambda e: e.tensor_copy(out=Q_s.ap[64:128, :], in_=tbank.ap[64:128, 0:512]), reads=[tbank], writes=[Q_s])

        build1(0)
        build2(0)
        for g in range(NG):
            Q_s, o_s = Qp[g % 2], osb[g % 2]
            cs = slice(g * 512, (g + 1) * 512)
            if g + 1 < NG:
                build1(g + 1)
            O = attention(p, A, lambda qs, Q_s=Q_s: Q_s.ap, [Q_s], lambda kt, K_s=K_s: K_s.ap[:, kt * 128:(kt + 1) * 128], K_s,
                          lambda kt, V_s=V_s: V_s.ap[:, kt, :], V_s, causal_tiles(g), 0.125)
            bcs = normalize(p, A, O)
            p.op("dve", lambda e, O=O, bcs=bcs, o_s=o_s: e.tensor_tensor(out=o_s.ap[:], in0=O.ap[0:64, :], in1=bcs.ap[:], op=ALU.mult), reads=[O, bcs], writes=[o_s])
            p.dma(oT.ap[h * 64:(h + 1) * 64, cs], o_s.ap[:], o_s, reads=[o_s], writes=[oT], q="pool")
            if g + 1 < NG:
                build2(g + 1)
    p.end_phase()


def phase_B_fox(p, cfg, cst, d, oT):
    S, NKT, NG = cfg.S, cfg.NKT, cfg.NG
    p.begin_phase()
    crow = d["crow"]
    CH = 2048
    lf = [p.sb([2, CH], F32, "lf") for _ in range(2)]
    cc = [p.sb([2, CH], F32, "cc") for _ in range(2)]
    t0 = p.sb([2, CH], F32, "t0")
    t1 = p.sb([2, CH], F32, "t1")
    R = [p.sb([2, 6, CH], BF16, "R") for _ in range(2)]
    for ch in range(S // CH):
        l_s, c_s, R_s = lf[ch % 2], cc[ch % 2], R[ch % 2]
        prev = cc[(ch + 1) % 2]
        p.dma(l_s.ap[:], d["logfT"].ap[:, ch * CH:(ch + 1) * CH], l_s, reads=[d["logfT"]], writes=[l_s])
        p.op("dve", lambda e, l_s=l_s: e.tensor_scalar(out=l_s.ap[:], in0=l_s.ap[:], scalar1=0.5, scalar2=None, op0=ALU.mult), reads=[l_s], writes=[l_s])
        if ch == 0:
            p.op("dve", lambda e, l_s=l_s, c_s=c_s: e.tensor_tensor_scan(out=c_s.ap[:], data0=l_s.ap[:], data1=l_s.ap[:], initial=0.0, op0=ALU.add, op1=ALU.add),
                 reads=[l_s], writes=[c_s])
        else:
            p.op("dve", lambda e, l_s=l_s, c_s=c_s, prev=prev: e.tensor_tensor_scan(out=c_s.ap[:], data0=l_s.ap[:], data1=l_s.ap[:], initial=prev.ap[:, CH - 1:CH], op0=ALU.add, op1=ALU.add),
                 reads=[l_s, prev], writes=[c_s])
        p.op("dve", lambda e, c_s=c_s: e.tensor_scalar(out=t0.ap[:], in0=c_s.ap[:], scalar1=8.0, scalar2=None, op0=ALU.mult), reads=[c_s], writes=[t0])
        for lvl in range(3):
            p.op("dve", lambda e, lvl=lvl, R_s=R_s: e.tensor_copy(out=R_s.ap[:, lvl, :], in_=t0.ap[:]), reads=[t0], writes=[R_s])
            if lvl < 2:
                p.op("dve", lambda e, lvl=lvl, R_s=R_s: e.tensor_copy(out=t1.ap[:], in_=R_s.ap[:, lvl, :]), reads=[R_s], writes=[t1])
                p.op("dve", lambda e: e.tensor_tensor(out=t0.ap[:], in0=t0.ap[:], in1=t1.ap[:], op=ALU.subtract), reads=[t0, t1], writes=[t0])
        p.op("dve", lambda e, R_s=R_s: e.tensor_scalar(out=R_s.ap[:, 3:6, :], in0=R_s.ap[:, 0:3, :], scalar1=-1.0, scalar2=None, op0=ALU.mult), reads=[R_s], writes=[R_s])
        p.dma(crow.ap[:, :, ch * CH:(ch + 1) * CH], R_s.ap[:], R_s, reads=[R_s], writes=[crow])
    p.end_phase()
    p.begin_phase()
    c = load_consts(p, cst)
    A = Attn(p, c, cst, n_s=3)
    Kp = [p.sb([70, S], BF16, "Kp") for _ in range(2)]
    Vs = [p.sb([128, NKT, 65], BF16, "Vs") for _ in range(2)]
    Qp = [p.sb([70, 512], BF16, "Qp") for _ in range(2)]
    osb = [p.sb([64, 512], BF16, "osb") for _ in range(2)]
    for k in Kp:
        p.op("pool", lambda e, k=k: e.memset(k.ap[64:67, :], 1.0), writes=[k])
    for q in Qp:
        p.dma(q.ap[67:70, :], cst["ones_row"].ap[:, :], q, writes=[q])
    it = 0
    for h in range(2):
        K_s, V_s = Kp[h % 2], Vs[h % 2]
        p.dma(K_s.ap[0:64, :], d["fkT"].ap[h * 64:(h + 1) * 64, :], K_s, reads=[d["fkT"]], writes=[K_s])
        p.dma(K_s.ap[67:70, :], crow.ap[h, 3:6, :], K_s, reads=[crow], writes=[K_s])
        p.dma(V_s.ap[:], d["fvp"].ap[h, :, :, :], V_s, reads=[d["fvp"]], writes=[V_s])
        for g in range(NG):
            Q_s, o_s = Qp[it % 2], osb[it % 2]
            it += 1
            cs = slice(g * 512, (g + 1) * 512)
            p.dma(Q_s.ap[0:64, :], d["fqT"].ap[h * 64:(h + 1) * 64, cs], Q_s, reads=[d["fqT"]], writes=[Q_s])
            p.dma(Q_s.ap[64:67, :], crow.ap[h, 0:3, cs], Q_s, reads=[crow], writes=[Q_s])
            O = attention(p, A, lambda qs, Q_s=Q_s: Q_s.ap, [Q_s], lambda kt, K_s=K_s: K_s.ap[:, kt * 128:(kt + 1) * 128], K_s,
                          lambda kt, V_s=V_s: V_s.ap[:, kt, :], V_s, causal_tiles(g), 0.125)
            bcs = normalize(p, A, O)
            p.op("dve", lambda e, O=O, bcs=bcs, o_s=o_s: e.tensor_tensor(out=o_s.ap[:], in0=O.ap[0:64, :], in1=bcs.ap[:], op=ALU.mult), reads=[O, bcs], writes=[o_s])
            p.dma(oT.ap[h * 64:(h + 1) * 64, cs], o_s.ap[:], o_s, reads=[o_s], writes=[oT], q="pool")
    p.end_phase()


def phase_B_nsa_compress(p, cfg, cst, d):
    S = cfg.S
    NCP = S // 16
    NCB = NCP - 1
    p.begin_phase()
    c = load_consts(p, cst)
    banks = [p.ps([128, 512], F32, "bank") for _ in range(4)]
    X = p.sb([64, S], BF16, "X")
    w1 = p.sb([64, 32, 256], BF16, "w1")
    w2 = p.sb([128, 2, 64], BF16, "w2")
    pe = p.sb([64, 32], BF16, "pe")
    st1 = [p.sb([64, 8, 256], F32, "st1") for _ in range(2)]
    st2 = p.sb([128, 2, 64], F32, "st2")
    st3 = p.sb([64, 32], F32, "st3")
    biasb = p.sb([128, 2], F32, "biasb")
    hid = [p.sb([128, NCP], BF16, "hid") for _ in range(2)]
    u = [p.sb([128, 512], F32, "u") for _ in range(2)]
    x2 = p.sb([128, 512], F32, "x2")
    x3 = p.sb([128, 512], F32, "x3")
    sg = p.sb([128, 512], F32, "sg")
    kc_s = p.sb([64, NCP], BF16, "kc_s")
    vc_s = p.sb([128, NCP // 128, 65], BF16, "vc_s")
    p.op("pool", lambda e: e.memset(vc_s.ap[:], 1.0), writes=[vc_s])
    bi = 0
    for (src, w1d, w2d, ped, isk) in [(d["kcT"], d["w1k"], d["w2k"], d["pekT"], True), (d["vcT"], d["w1v"], d["w2v"], d["pevT"], False)]:
        p.dma(X.ap[:], src.ap[:, :], X, reads=[src], writes=[X])
        for i in range(4):
            s_ = st1[i % 2]
            p.dma(s_.ap[:], w1d.ap[i * 512:(i + 1) * 512, :].rearrange("(p d) h -> d p h", d=64), s_, writes=[s_])
            p.op("pool", lambda e, s_=s_, i=i: e.tensor_copy(out=w1.ap[:, i * 8:(i + 1) * 8, :], in_=s_.ap[:]), reads=[s_], writes=[w1])
        p.dma(st2.ap[:], w2d.ap[:, :].rearrange("(c p) d -> p c d", p=128), st2, writes=[st2])
        p.op("pool", lambda e: e.tensor_copy(out=w2.ap[:], in_=st2.ap[:]), reads=[st2], writes=[w2])
        p.dma(st3.ap[:], ped.ap[:, :], st3, writes=[st3])
        p.op("pool", lambda e: e.tensor_copy(out=pe.ap[:], in_=st3.ap[:]), reads=[st3], writes=[pe])
        bk = banks[bi % 4]
        bi += 1
        for hc in range(2):
            for pos in range(32):
                p.op("pe", lambda e, hc=hc, pos=pos, bk=bk: e.matmul(bk.ap[:, hc:hc + 1], lhsT=w1.ap[:, pos, hc * 128:(hc + 1) * 128], rhs=pe.ap[:, pos:pos + 1],
                                                                    start=(pos == 0), stop=(pos == 31)), reads=[w1, pe], writes=[bk], inc=(pos == 31))
        p.op("act", lambda e, bk=bk: e.activation(out=biasb.ap[:], in_=bk.ap[:, 0:2], func=AF.Copy), reads=[bk], writes=[biasb])
        for hh in hid:
            p.op("pool", lambda e, hh=hh: e.memset(hh.ap[:], 0.0), writes=[hh])
        for hc in range(2):
            for cb in range((NCB + 511) // 512):
                n = min(512, NCB - cb * 512)
                bk = banks[bi % 4]
                u_s = u[bi % 2]
                bi += 1
                for pos in range(32):
                    a0 = pos + 16 * cb * 512
                    p.op("pe", lambda e, hc=hc, pos=pos, bk=bk, a0=a0, n=n: e.matmul(bk.ap[:, 0:n], lhsT=w1.ap[:, pos, hc * 128:(hc + 1) * 128],
                                                                                     rhs=X.ap[:, a0:a0 + 16 * (n - 1) + 1:16], start=(pos == 0), stop=(pos == 31)),
                         reads=[w1, X], writes=[bk], inc=(pos == 31))
                p.op("act", lambda e, bk=bk, u_s=u_s, n=n, hc=hc: e.activation(out=u_s.ap[:, 0:n], in_=bk.ap[:, 0:n], func=AF.Identity, bias=biasb.ap[:, hc:hc + 1]),
                     reads=[bk, biasb], writes=[u_s])
                p.op("dve", lambda e, u_s=u_s, n=n: e.tensor_tensor(out=x2.ap[:, 0:n], in0=u_s.ap[:, 0:n], in1=u_s.ap[:, 0:n], op=ALU.mult), reads=[u_s], writes=[x2])
                p.op("dve", lambda e, u_s=u_s, n=n: e.tensor_tensor(out=x3.ap[:, 0:n], in0=x2.ap[:, 0:n], in1=u_s.ap[:, 0:n], op=ALU.mult), reads=[u_s, x2], writes=[x3])
                p.op("dve", lambda e, u_s=u_s, n=n: e.scalar_tensor_tensor(out=x2.ap[:, 0:n], in0=x3.ap[:, 0:n], scalar=0.044715, in1=u_s.ap[:, 0:n], op0=ALU.mult, op1=ALU.add),
                     reads=[u_s, x3], writes=[x2])
                p.op("act", lambda e, n=n: e.activation(out=sg.ap[:, 0:n], in_=x2.ap[:, 0:n], func=AF.Sigmoid, scale=1.5957691216057308), reads=[x2], writes=[sg])
                p.op("dve", lambda e, u_s=u_s, n=n, hc=hc, cb=cb: e.tensor_tensor(out=hid[hc].ap[:, cb * 512:cb * 512 + n], in0=u_s.ap[:, 0:n], in1=sg.ap[:, 0:n], op=ALU.mult),
                     reads=[u_s, sg], writes=[hid[hc]])
        if isk:
            for cb in range(NCP // 512 if NCP >= 512 else 1):
                n = min(512, NCP)
                bk = banks[bi % 4]
                bi += 1
                for hc in range(2):
                    p.op("pe", lambda e, hc=hc, bk=bk, cb=cb, n=n: e.matmul(bk.ap[0:64, 0:n], lhsT=w2.ap[:, hc, :], rhs=hid[hc].ap[:, cb * 512:cb * 512 + n], start=(hc == 0), stop=(hc == 1)),
                         reads=[w2, hid[hc]], writes=[bk], inc=(hc == 1))
                p.op("act", lambda e, bk=bk, cb=cb, n=n: e.activation(out=kc_s.ap[:, cb * 512:cb * 512 + n], in_=bk.ap[0:64, 0:n], func=AF.Copy), reads=[bk], writes=[kc_s])
            p.dma(d["kcmpT"].ap[:, :], kc_s.ap[:], kc_s, reads=[kc_s], writes=[d["kcmpT"]])
        else:
            for bt in range(NCP // 128):
                bk = banks[bi % 4]
                bi += 1
                for hc in range(2):
                    p.op("pe", lambda e, hc=hc, bk=bk, bt=bt: e.matmul(bk.ap[:, 0:64], lhsT=hid[hc].ap[:, bt * 128:(bt + 1) * 128], rhs=w2.ap[:, hc, :], start=(hc == 0), stop=(hc == 1)),
                         reads=[w2, hid[hc]], writes=[bk], inc=(hc == 1))
                p.op("act", lambda e, bk=bk, bt=bt: e.activation(out=vc_s.ap[:, bt, 0:64], in_=bk.ap[:, 0:64], func=AF.Copy), reads=[bk], writes=[vc_s])
            p.dma(d["vcmp"].ap[:, :, :], vc_s.ap[:], vc_s, reads=[vc_s], writes=[d["vcmp"]])
    p.end_phase()


def phase_B_nsa(p, cfg, cst, d, oT):
    S, NKT, NG = cfg.S, cfg.NKT, cfg.NG
    NCP = S // 16
    NSB = S // 64
    NCC = max(1, NSB // 64)
    NSBW = min(NSB, 64)
    p.begin_phase()
    c = load_consts(p, cst)
    A = Attn(p, c, cst, n_s=2)
    Sc = p.ps([128, 1024], F32, "Sc")
    tb = p.ps([128, 1024], BF16, "tb")
    Ksel = p.sb([128, S], BF16, "Ksel")
    Kwin = p.sb([64, S], BF16, "Kwin")
    Kcmp = p.sb([64, NCP], BF16, "Kcmp")
    Vsel = p.sb([128, NKT, 65], BF16, "Vsel")
    Vwin = p.sb([128, NKT, 65], BF16, "Vwin")
    Vcmp = p.sb([128, NCP // 128, 65], BF16, "Vcmp")
    p.dma(Ksel.ap[0:64, :], d["ksT"].ap[:, :], Ksel, reads=[d["ksT"]], writes=[Ksel])
    p.dma(Ksel.ap[64:128, :], cst["nsa_oh"].ap[:, :], Ksel, writes=[Ksel])
    p.dma(Kwin.ap[:], d["kwT"].ap[:, :], Kwin, reads=[d["kwT"]], writes=[Kwin])
    p.dma(Kcmp.ap[:], d["kcmpT"].ap[:, :], Kcmp, reads=[d["kcmpT"]], writes=[Kcmp])
    p.dma(Vsel.ap[:], d["vsp"].ap[:, :, :], Vsel, reads=[d["vsp"]], writes=[Vsel])
    p.dma(Vwin.ap[:], d["vwp"].ap[:, :, :], Vwin, reads=[d["vwp"]], writes=[Vwin])
    p.dma(Vcmp.ap[:], d["vcmp"].ap[:, :, :], Vcmp, reads=[d["vcmp"]], writes=[Vcmp])
    m01 = p.sb([128, 2064], BF16, "m01")
    p.dma(m01.ap[:], cst["nsa_m01"].ap[:, :], m01, writes=[m01])
    sbase = p.sb([128, 512], F32, "sbase")
    p.dma(sbase.ap[:], cst["nsa_stat"].ap[:, :], sbase, writes=[sbase])
    Qp = [[p.sb([128, 512], BF16, "Qp") for _ in range(NCC)] for _ in range(2)]
    Qi = [p.sb([64, 4, 512], BF16, "Qi") for _ in range(2)]
    G = p.sb([65, 6, 512], F32, "G")
    e32 = p.sb([128, 1024], F32, "e32")
    em = p.sb([128, 1024], F32, "em")
    Pb = p.sb([128, 1032], F32, "Pb")
    p.op("pool", lambda e: e.memset(Pb.ap[:], 0.0), writes=[Pb])
    imp = p.sb([128, 256], F32, "imp")
    p.op("pool", lambda e: e.memset(imp.ap[:], 0.0), writes=[imp])
    rs = p.sb([128, 4], F32, "rs")
    vv = p.sb([128, 256], F32, "vv")
    vv2 = p.sb([128, 256], F32, "vv2")
    m8a = p.sb([128, 8], F32, "m8a")
    m8b = p.sb([128, 8], F32, "m8b")
    thr = p.sb([128, 1], F32, "thr")
    mt = [p.sb([128, NCC, 4, 128], BF16, "mt") for _ in range(2)]
    for m in mt:
        p.op("pool", lambda e, m=m: e.memset(m.ap[:], 0.0), writes=[m])
    acc = p.sb([64, 512], F32, "acc")
    tmpo = p.sb([64, 512], F32, "tmpo")
    osb = [p.sb([64, 512], BF16, "osb") for _ in range(2)]

    def build1(g):
        Qi_s, mt_s = Qi[g % 2], mt[g % 2]
        cs = slice(g * 512, (g + 1) * 512)
        p.dma(Qi_s.ap[:], d["nqT"].ap[:, cs].rearrange("(h p) n -> p h n", p=64), Qi_s, reads=[d["nqT"]], writes=[Qi_s])
        for sub in range(4):
            m = 4 * g + sub
            ncw = min(NCP, ((8 * (m + 1) + 255) // 256) * 256)
            nsw = ncw // 4
            for h in range(4):
                for cb in range((ncw + 511) // 512):
                    n = min(512, ncw - cb * 512)
                    p.op("pe", lambda e, h=h, cb=cb, n=n, sub=sub: e.matmul(Sc.ap[:, cb * 512:cb * 512 + n], lhsT=Qi_s.ap[:, h, sub * 128:(sub + 1) * 128],
                                                                         rhs=Kcmp.ap[:, cb * 512:cb * 512 + n], start=True, stop=True),
                         reads=[Qi_s, Kcmp], writes=[Sc])
                p.op("act", lambda e, ncw=ncw: e.activation(out=e32.ap[:, 0:ncw], in_=Sc.ap[:, 0:ncw], func=AF.Exp, scale=0.125), reads=[Sc], writes=[e32])
                p.op("dve", lambda e, ncw=ncw, m=m: e.tensor_tensor(out=em.ap[:, 0:ncw], in0=e32.ap[:, 0:ncw], in1=m01.ap[:, 1024 - 8 * m:1024 - 8 * m + ncw], op=ALU.mult),
                     reads=[e32, m01], writes=[em])
                p.op("dve", lambda e, ncw=ncw, h=h: e.tensor_reduce(out=rs.ap[:, h:h + 1], in_=em.ap[:, 0:ncw], axis=AX.X, op=ALU.add), reads=[em], writes=[rs])
                p.op("dve", lambda e, h=h: e.tensor_scalar(out=rs.ap[:, h:h + 1], in0=rs.ap[:, h:h + 1], scalar1=1e-30, scalar2=None, op0=ALU.max), reads=[rs], writes=[rs])
                p.op("dve", lambda e, h=h: e.reciprocal(out=rs.ap[:, h:h + 1], in_=rs.ap[:, h:h + 1]), reads=[rs], writes=[rs])
                if h == 0:
                    p.op("dve", lambda e, ncw=ncw: e.tensor_scalar(out=Pb.ap[:, 1:1 + ncw], in0=em.ap[:, 0:ncw], scalar1=rs.ap[:, 0:1], scalar2=None, op0=ALU.mult),
                         reads=[em, rs], writes=[Pb])
                else:
                    p.op("dve", lambda e, ncw=ncw, h=h: e.scalar_tensor_tensor(out=Pb.ap[:, 1:1 + ncw], in0=em.ap[:, 0:ncw], scalar=rs.ap[:, h:h + 1], in1=Pb.ap[:, 1:1 + ncw],
                                                                               op0=ALU.mult, op1=ALU.add), reads=[em, rs, Pb], writes=[Pb])
            p.op("dve", lambda e, nsw=nsw: e.tensor_reduce(out=imp.ap[:, 0:nsw], in_=Pb.ap[:, 0:4 * nsw].rearrange("p (a b) -> p a b", b=4), axis=AX.X, op=ALU.add),
                 reads=[Pb], writes=[imp])
            p.op("dve", lambda e, nsw=nsw: e.tensor_tensor(out=imp.ap[:, 0:nsw], in0=imp.ap[:, 0:nsw], in1=Pb.ap[:, 4:4 * nsw + 1:4], op=ALU.add), reads=[Pb, imp], writes=[imp])
            p.op("dve", lambda e, m=m: e.tensor_tensor(out=vv.ap[:, 0:NSB], in0=imp.ap[:, 0:NSB], in1=sbase.ap[:, 256 - 2 * m:256 - 2 * m + NSB], op=ALU.add),
                 reads=[imp, sbase], writes=[vv])
            p.op("dve", lambda e: e.memset(vv.ap[:, 0:1], 1e9), writes=[vv])
            p.op("dve", lambda e: e.max(out=m8a.ap[:], in_=vv.ap[:, 0:NSB]), reads=[vv], writes=[m8a])
            p.op("dve", lambda e: e.match_replace(out=vv2.ap[:, 0:NSB], in_to_replace=m8a.ap[:], in_values=vv.ap[:, 0:NSB], imm_value=-3e38), reads=[vv, m8a], writes=[vv2])
            p.op("dve", lambda e: e.max(out=m8b.ap[:], in_=vv2.ap[:, 0:NSB]), reads=[vv2], writes=[m8b])
            p.op("dve", lambda e: e.tensor_scalar(out=thr.ap[:], in0=m8b.ap[:, 7:8], scalar1=-1e8, scalar2=None, op0=ALU.max), reads=[m8b], writes=[thr])
            p.op("dve", lambda e, sub=sub: e.tensor_scalar(out=mt_s.ap[:, :, sub, 64:64 + NSBW], in0=vv.ap[:, 0:NSB].rearrange("p (c j) -> p c j", j=NSBW), scalar1=thr.ap[:, 0:1], scalar2=-1.0,
                                                           op0=ALU.is_ge, op1=ALU.add), reads=[vv, thr], writes=[mt_s])

    def build2(g):
        mt_s = mt[g % 2]
        cs = slice(g * 512, (g + 1) * 512)
        for hh in range(2):
            for cc in range(NCC):
                p.dma(Qp[hh][cc].ap[0:64, :], d["nqT"].ap[hh * 64:(hh + 1) * 64, cs], Qp[hh][cc], reads=[d["nqT"]], writes=[Qp[hh][cc]])
        p.dma(G.ap[64:65, :, :], d["gatesT"].ap[:, cs].rearrange("(o r) n -> o r n", o=1), G, reads=[d["gatesT"]], writes=[G])
        for half in range(2):
            for cc in range(NCC):
                for s2 in range(2):
                    sub = half * 2 + s2
                    p.op("pe", lambda e, cc=cc, sub=sub, s2=s2: e.transpose(out=tb.ap[:, (cc * 2 + s2) * 128:(cc * 2 + s2 + 1) * 128], in_=mt_s.ap[:, cc, sub, :], identity=c.ident.ap[:]),
                         reads=[mt_s, c.ident], writes=[tb], inc=(cc == NCC - 1 and s2 == 1))
            for cc in range(NCC):
                for hh in range(2):
                    p.op("dve", lambda e, cc=cc, hh=hh, half=half: e.tensor_copy(out=Qp[hh][cc].ap[64:128, half * 256:(half + 1) * 256], in_=tb.ap[64:128, cc * 256:(cc + 1) * 256]),
                         reads=[tb], writes=[Qp[hh][cc]])

    def branch_tiles(g, kind):
        if kind == "cmp":
            tl = []
            for bt in range(NCP // 128):
                dl = 512 * g - 2048 * bt
                if dl < 0:
                    continue
                if dl >= 2560:
                    tl.append((bt, None, 0, 512, 0))
                else:
                    tl.append((bt, 8 + dl // 512, 0, 512, 0))
            return tl
        if kind == "sel":
            return causal_tiles(g, lambda kt: min(kt // 32, NCC - 1))
        tl = [(4 * g + dd, dd, 128 * dd, 512, 0) for dd in range(4)]
        if g > 0:
            tl += [(4 * (g - 1) + mm, 4 + mm, 0, 128 * (mm + 1), 0) for mm in range(4)]
        return tl

    build1(0)
    build2(0)
    it = 0
    for g in range(NG):
        cs = slice(g * 512, (g + 1) * 512)
        if g + 1 < NG:
            build1(g + 1)
        for hh in range(2):
            o_s = osb[it % 2]
            it += 1
            for bi_, kind in enumerate(("cmp", "sel", "win")):
                tl = branch_tiles(g, kind)
                if kind == "cmp":
                    O = attention(p, A, lambda qs, hh=hh: Qp[hh][0].ap[0:64, :], [Qp[hh][0]], lambda kt: Kcmp.ap[:, kt * 128:(kt + 1) * 128], Kcmp,
                                  lambda kt: Vcmp.ap[:, kt, :], Vcmp, tl, 0.125)
                elif kind == "sel":
                    O = attention(p, A, lambda qs, hh=hh: Qp[hh][qs].ap, Qp[hh], lambda kt: Ksel.ap[:, kt * 128:(kt + 1) * 128], Ksel,
                                  lambda kt: Vsel.ap[:, kt, :], Vsel, tl, 0.125)
                else:
                    O = attention(p, A, lambda qs, hh=hh: Qp[hh][0].ap[0:64, :], [Qp[hh][0]], lambda kt: Kwin.ap[:, kt * 128:(kt + 1) * 128], Kwin,
                                  lambda kt: Vwin.ap[:, kt, :], Vwin, tl, 0.125)
                bcs = normalize(p, A, O, gate_ap=G.ap[64:65, hh * 3 + bi_, :], gate_buf=G)
                if bi_ == 0:
                    p.op("dve", lambda e, O=O, bcs=bcs: e.tensor_tensor(out=acc.ap[:], in0=O.ap[0:64, :], in1=bcs.ap[:], op=ALU.mult), reads=[O, bcs], writes=[acc])
                else:
                    p.op("dve", lambda e, O=O, bcs=bcs: e.tensor_tensor(out=tmpo.ap[:], in0=O.ap[0:64, :], in1=bcs.ap[:], op=ALU.mult), reads=[O, bcs], writes=[tmpo])
                    if bi_ == 1:
                        p.op("pool", lambda e: e.tensor_tensor(out=acc.ap[:], in0=acc.ap[:], in1=tmpo.ap[:], op=ALU.add), reads=[acc, tmpo], writes=[acc])
                    else:
                        p.op("pool", lambda e, o_s=o_s: e.tensor_tensor(out=o_s.ap[:], in0=acc.ap[:], in1=tmpo.ap[:], op=ALU.add), reads=[acc, tmpo], writes=[o_s])
            p.dma(oT.ap[128 + hh * 64:128 + (hh + 1) * 64, cs], o_s.ap[:], o_s, reads=[o_s], writes=[oT], q="pool")
        if g + 1 < NG:
            build2(g + 1)
    p.end_phase()


def _bf(a):
    return np.ascontiguousarray(a).astype(NPBF)


def host_consts(cfg):
    S = cfg.S
    cst = {}
    cst["ident"] = _bf(np.eye(128, dtype=np.float32))
    mp = np.zeros((128, 13, 512), np.float32)
    sl = np.arange(128)[:, None]
    tl = np.arange(512)[None, :]
    for dd in range(4):
        mp[:, dd, :] = np.where(128 * dd + sl <= tl, 0.0, NEG)
    for mm in range(4):
        mp[:, 4 + mm, :] = np.where(sl > tl - 128 * mm, 0.0, NEG)
    for i in range(5):
        mp[:, 8 + i, :] = np.where(16 * sl + 31 - tl <= 512 * i, 0.0, NEG)
    cst["mpat"] = _bf(mp)
    x = np.arange(128)[None, :] - 64
    st = np.where(x < 0, 0.0, np.where(x == 0, 1e30, -1e30)).astype(np.float32)
    cst["moba_stat"] = np.ascontiguousarray(np.broadcast_to(st, (128, 128))).astype(np.float32)
    s = np.arange(S)
    cst["moba_oh"] = _bf(np.where((s[None, :] // 256) == np.arange(64)[:, None], 30000.0, 0.0))
    cst["nsa_oh"] = _bf(np.where(((s[None, :] // 64) % 64) == np.arange(64)[:, None], 30000.0, 0.0))
    xx = np.arange(2064)[None, :]
    pp = np.arange(128)[:, None]
    cst["nsa_m01"] = _bf(np.where(16 * (xx - 1024) <= pp - 31, 1.0, 0.0))
    jj = np.arange(512)[None, :] - 256
    qb = (pp >= 64).astype(np.int64)
    cst["nsa_stat"] = np.where((jj == qb) | (jj == qb - 1), 1e9, np.where(jj > qb, -1e9, 0.0)).astype(np.float32)
    cst["ones_row"] = _bf(np.ones((3, 512), np.float32))
    return cst


def rope_tables(cfg, core):
    r = core % 4
    pos = (r * cfg.TPC + np.arange(cfg.TPC)).astype(np.float32)
    inv = (10000.0 ** (-np.arange(0, HD, 2, dtype=np.float32) / HD)).astype(np.float32)
    ang = pos[None, :] * inv[:, None]
    cos = np.cos(ang).astype(np.float32)
    sin = np.sin(ang).astype(np.float32)
    cosT = np.tile(cos, (4, 1))
    sgn = np.where((np.arange(128) % 64) < 32, -1.0, 1.0).astype(np.float32)[:, None]
    sinT = np.tile(sin, (4, 1)) * sgn
    return np.ascontiguousarray(cosT), np.ascontiguousarray(sinT.astype(np.float32))


def rot_cols(w):
    K, N = w.shape
    return np.ascontiguousarray(w.reshape(K, N // 64, 2, 32)[:, :, ::-1, :].reshape(K, N))


def gains_layout(g_post, g_fpre, g_fpost, g_pre):
    g = np.stack([g_post, g_fpre, g_fpost, g_pre], 0)
    return np.ascontiguousarray(g.reshape(4, 8, 128).transpose(2, 0, 1)).astype(np.float32)


class Launch:
    def __init__(self):
        self.p = Prog()
        self.in_common = {}
        self.in_percore = [dict() for _ in range(NCORE)]
        self.outs = []

    def inp(self, name, arr_or_list):
        if isinstance(arr_or_list, list):
            a0 = arr_or_list[0]
            for i in range(NCORE):
                self.in_percore[i][name] = np.ascontiguousarray(arr_or_list[i])
        else:
            a0 = arr_or_list
            self.in_common[name] = np.ascontiguousarray(a0)
        dt = BF16 if a0.dtype == NPBF else F32
        return self.p.dram(name, list(a0.shape), dt, "ExternalInput")

    def out(self, name, shape, dt):
        self.outs.append(name)
        return self.p.dram(name, list(shape), dt, "ExternalOutput")

    def scratch(self, name, shape, dt):
        return self.p.dram(name, list(shape), dt, "Internal")

    def consts(self, cst):
        return {k: self.inp("c_" + k, v) for k, v in cst.items()}

    def run(self):
        self.p.finish()
        in_maps = []
        for i in range(NCORE):
            m = dict(self.in_common)
            m.update(self.in_percore[i])
            in_maps.append(m)
        res = run_bass_kernel_spmd(self.p.nc, in_maps, core_ids=list(range(NCORE)))
        return [{k: np.asarray(r[k]) for k in self.outs} for r in res.results]


def cat_tok(res, name, b, rows=None):
    parts = [res[4 * b + r][name] for r in range(4)]
    if rows is not None:
        parts = [x[rows] for x in parts]
    return np.ascontiguousarray(np.concatenate(parts, axis=-1))


def run_layer_A(cfg, cst, even, xT_list, W, gains, bf=None):
    L = Launch()
    p = L.p
    TPC, LKT = cfg.TPC, cfg.LKT
    cd = L.consts(cst)
    xT = L.inp("xT", xT_list)
    Wd = L.inp("W", W)
    gd = L.inp("gains", gains)
    tabs = [rope_tables(cfg, i) for i in range(NCORE)]
    cosT = L.inp("cosT", [t[0] for t in tabs])
    sinT = L.inp("sinT", [t[1] for t in tabs])
    outs = {}
    if even:
        bfd = L.inp("bf", bf)
        outs["fqT"] = L.out("fqT", [512, TPC], BF16)
        outs["fkT"] = L.out("fkT", [512, TPC], BF16)
        outs["nqT"] = L.out("nqT", [512, TPC], BF16)
        outs["nkT"] = L.out("nkT", [384, TPC], BF16)
        outs["vcT"] = L.out("vcT", [128, TPC], BF16)
        outs["logfT"] = L.out("logfT", [8, TPC], F32)
        outs["gatesT"] = L.out("gatesT", [24, TPC], F32)
        outs["vp"] = L.out("vp", [12, 128, LKT, 65], BF16)
    else:
        bfd = None
        outs["qT"] = L.out("qT", [1024, TPC], BF16)
        outs["kT"] = L.out("kT", [1024, TPC], BF16)
        outs["vp"] = L.out("vp", [16, 128, LKT, 65], BF16)
    phase_A(p, cfg, cd, even, xT, Wd, W.shape[1], gd, cosT, sinT, bfd, outs)
    for o in outs.values():
        p.out_events.update(o.w)
    return L.run()


def run_layer_C(cfg, cst, oT_list, xT_list, w_out, w_gate, w_up, w_down, gains):
    L = Launch()
    p = L.p
    TPC = cfg.TPC
    cd = L.consts(cst)
    oT = L.inp("oT", oT_list)
    xT = L.inp("xT", xT_list)
    wo = L.inp("w_out", w_out)
    wg = L.inp("w_gate", w_gate)
    wu = L.inp("w_up", w_up)
    wd = L.inp("w_down", w_down)
    gd = L.inp("gains", gains)
    xo = L.out("xT_out", [1024, TPC], F32)
    hT = L.scratch("hT_s", [1024, TPC], BF16)
    mT = L.scratch("mT_s", [DFF, TPC], BF16)
    x1 = L.scratch("x1_s", [1024, TPC], F32)
    phase_C(p, cfg, cd, oT, xT, xo, wo, wg, wu, wd, gd, hT, mT, x1, True)
    return L.run()


def run_B_moba(cfg, cst, qT_l, kT_l, vp_l):
    L = Launch()
    cd = L.consts(cst)
    qT = L.inp("qT", qT_l)
    kT = L.inp("kT", kT_l)
    vp = L.inp("vp", vp_l)
    oT = L.out("oT", [256, cfg.S], BF16)
    phase_B_moba(L.p, cfg, cd, qT, kT, vp, oT)
    L.p.out_events.update(oT.w)
    return L.run()


def run_B_even(cfg, cst, dl):
    L = Launch()
    S = cfg.S
    cd = L.consts(cst)
    d = {k: L.inp(k, v) for k, v in dl.items()}
    d["crow"] = L.scratch("crow", [2, 6, S], BF16)
    d["kcmpT"] = L.scratch("kcmpT", [64, S // 16], BF16)
    d["vcmp"] = L.scratch("vcmp", [128, S // 16 // 128, 65], BF16)
    oT = L.out("oT", [256, S], BF16)
    phase_B_fox(L.p, cfg, cd, d, oT)
    phase_B_nsa_compress(L.p, cfg, cd, d)
    phase_B_nsa(L.p, cfg, cd, d, oT)
    L.p.out_events.update(oT.w)
    return L.run()


def run_C_then_A(cfg, cst, oT_list, xT_list, w_out, w_gate, w_up, w_down, gainsC, W_next, gainsA):
    L = Launch()
    p = L.p
    TPC, LKT = cfg.TPC, cfg.LKT
    cd = L.consts(cst)
    oT = L.inp("oT", oT_list)
    xT = L.inp("xT", xT_list)
    wo = L.inp("w_out", w_out)
    wg = L.inp("w_gate", w_gate)
    wu = L.inp("w_up", w_up)
    wd = L.inp("w_down", w_down)
    gC = L.inp("gainsC", gainsC)
    xo = L.out("xT_out", [1024, TPC], F32)
    hT = L.scratch("hT_s", [1024, TPC], BF16)
    mT = L.scratch("mT_s", [DFF, TPC], BF16)
    x1 = L.scratch("x1_s", [1024, TPC], F32)
    phase_C(p, cfg, cd, oT, xT, xo, wo, wg, wu, wd, gC, hT, mT, x1, True)
    Wd = L.inp("W", W_next)
    gA = L.inp("gainsA", gainsA)
    tabs = [rope_tables(cfg, i) for i in range(NCORE)]
    cosT = L.inp("cosT", [t[0] for t in tabs])
    sinT = L.inp("sinT", [t[1] for t in tabs])
    outs = {"qT": L.out("qT", [1024, TPC], BF16), "kT": L.out("kT", [1024, TPC], BF16), "vp": L.out("vp", [16, 128, LKT, 65], BF16)}
    phase_A(p, cfg, cd, False, xo, Wd, W_next.shape[1], gA, cosT, sinT, None, outs)
    for o in outs.values():
        p.out_events.update(o.w)
    return L.run()


def attn_to_token_shards(cfg, resB, even):
    TPC = cfg.TPC
    oT_l = []
    for i in range(NCORE):
        b, r = divmod(i, 4)
        cols = slice(r * TPC, (r + 1) * TPC)
        if even:
            fox = [resB[4 * b + hg]["oT"][0:128, cols] for hg in range(4)]
            nsa = [resB[4 * b + hg]["oT"][128:256, cols] for hg in range(4)]
            oT_l.append(np.ascontiguousarray(np.concatenate(fox + nsa, axis=0)))
        else:
            oT_l.append(np.ascontiguousarray(np.concatenate([resB[4 * b + hg]["oT"][:, cols] for hg in range(4)], axis=0)))
    return oT_l

def head_shard(res, name, b, row_sl):
    return cat_tok(res, name, b, rows=row_sl)


def forward(cfg, x, ev_w_in, ev_b_f, ev_cmp_pe_k, ev_cmp_w1_k, ev_cmp_w2_k, ev_cmp_pe_v, ev_cmp_w1_v, ev_cmp_w2_v,
            ev_w_out, od_w_in, od_w_out, g_mix_pre, g_mix_post, g_ffn_pre, g_ffn_post, ffn_w_gate, ffn_w_up, ffn_w_down, debug=None):
    S, TPC, LKT = cfg.S, cfg.TPC, cfg.LKT
    cst = host_consts(cfg)
    f32 = lambda a: np.ascontiguousarray(np.asarray(a, dtype=np.float32))
    x = f32(x)
    xT = [np.ascontiguousarray(x[i // 4, (i % 4) * TPC:(i % 4 + 1) * TPC, :].T) for i in range(NCORE)]
    w = f32(ev_w_in)[0]
    offs = np.cumsum([0] + EVEN_SPLITS)
    pc = {n: w[:, offs[i]:offs[i + 1]] for i, n in enumerate(["fq", "fk", "fv", "fl", "nq", "kc", "vc", "ks", "vs", "kw", "vw", "gl"])}
    W0 = np.zeros((1024, 3840), np.float32)
    W0[:, 0:512] = pc["fq"]
    W0[:, 512:1024] = pc["fk"]
    W0[:, 1024:1536] = pc["nq"]
    W0[:, 1536:2048] = rot_cols(pc["nq"])
    W0[:, 2048:2176] = pc["kc"]
    W0[:, 2176:2304] = pc["ks"]
    W0[:, 2304:2432] = pc["kw"]
    W0[:, 2432:2560] = rot_cols(pc["kc"])
    W0[:, 2560:2688] = rot_cols(pc["ks"])
    W0[:, 2688:2816] = rot_cols(pc["kw"])
    W0[:, 2816:2944] = pc["vc"]
    W0[:, 2944:2952] = pc["fl"]
    W0[:, 2976:3000] = pc["gl"]
    W0[:, 3072:3584] = pc["fv"]
    W0[:, 3584:3712] = pc["vs"]
    W0[:, 3712:3840] = pc["vw"]
    gains0 = gains_layout(f32(g_mix_post)[0], f32(g_ffn_pre)[0], f32(g_ffn_post)[0], f32(g_mix_pre)[0])
    resA = run_layer_A(cfg, cst, True, xT, W0, gains0, bf=f32(ev_b_f)[0].reshape(8, 1))
    if debug is not None:
        debug["A0"] = resA
    dl = {k: [] for k in ["fqT", "fkT", "logfT", "fvp", "nqT", "kcT", "ksT", "kwT", "vcT", "vsp", "vwp", "gatesT"]}
    for i in range(NCORE):
        b, hg = divmod(i, 4)
        gq, hm = divmod(hg, 2)
        dl["fqT"].append(cat_tok(resA, "fqT", b, slice(hg * 128, (hg + 1) * 128)))
        dl["fkT"].append(cat_tok(resA, "fkT", b, slice(hg * 128, (hg + 1) * 128)))
        dl["logfT"].append(cat_tok(resA, "logfT", b, slice(2 * hg, 2 * hg + 2)))
        vpb = np.concatenate([resA[4 * b + r]["vp"] for r in range(4)], axis=2)
        dl["fvp"].append(np.ascontiguousarray(vpb[2 * hg:2 * hg + 2]))
        dl["vsp"].append(np.ascontiguousarray(vpb[8 + gq]))
        dl["vwp"].append(np.ascontiguousarray(vpb[10 + gq]))
        nq = cat_tok(resA, "nqT", b, slice(gq * 256, (gq + 1) * 256)).reshape(4, 64, S)
        order = [2 * hm, 2 * hm + 1, 2 * (1 - hm), 2 * (1 - hm) + 1]
        dl["nqT"].append(np.ascontiguousarray(nq[order].reshape(256, S)))
        dl["kcT"].append(cat_tok(resA, "nkT", b, slice(gq * 64, (gq + 1) * 64)))
        dl["ksT"].append(cat_tok(resA, "nkT", b, slice(128 + gq * 64, 128 + (gq + 1) * 64)))
        dl["kwT"].append(cat_tok(resA, "nkT", b, slice(256 + gq * 64, 256 + (gq + 1) * 64)))
        dl["vcT"].append(cat_tok(resA, "vcT", b, slice(gq * 64, (gq + 1) * 64)))
        hq = 4 * gq + 2 * hm
        dl["gatesT"].append(cat_tok(resA, "gatesT", b, slice(3 * hq, 3 * hq + 6)))
    dl["w1k"] = f32(ev_cmp_w1_k)[0]
    dl["w2k"] = f32(ev_cmp_w2_k)[0]
    dl["pekT"] = np.ascontiguousarray(f32(ev_cmp_pe_k)[0].T)
    dl["w1v"] = f32(ev_cmp_w1_v)[0]
    dl["w2v"] = f32(ev_cmp_w2_v)[0]
    dl["pevT"] = np.ascontiguousarray(f32(ev_cmp_pe_v)[0].T)
    resB = run_B_even(cfg, cst, dl)
    if debug is not None:
        debug["B0"] = resB
    w1 = f32(od_w_in)[0]
    q1, k1, v1 = w1[:, 0:1024], w1[:, 1024:2048], w1[:, 2048:3072]
    W1 = np.ascontiguousarray(np.concatenate([q1, rot_cols(q1), k1, rot_cols(k1), v1], axis=1))
    gains1 = gains_layout(f32(g_mix_post)[1], f32(g_ffn_pre)[1], f32(g_ffn_post)[1], f32(g_mix_pre)[1])
    resCA = run_C_then_A(cfg, cst, attn_to_token_shards(cfg, resB, True), xT, f32(ev_w_out)[0], f32(ffn_w_gate)[0], f32(ffn_w_up)[0],
                         f32(ffn_w_down)[0], gains0, W1, gains1)
    xT = [r["xT_out"] for r in resCA]
    if debug is not None:
        debug["x1"] = xT
        debug["A1"] = resCA
    qT_l, kT_l, vp_l = [], [], []
    for i in range(NCORE):
        b, hg = divmod(i, 4)
        qT_l.append(cat_tok(resCA, "qT", b, slice(hg * 256, (hg + 1) * 256)))
        kT_l.append(cat_tok(resCA, "kT", b, slice(hg * 256, (hg + 1) * 256)))
        vp_l.append(np.ascontiguousarray(np.concatenate([resCA[4 * b + r]["vp"][4 * hg:4 * hg + 4] for r in range(4)], axis=2)))
    resB1 = run_B_moba(cfg, cst, qT_l, kT_l, vp_l)
    if debug is not None:
        debug["B1"] = resB1
    xT = layer_post(cfg, cst, resB1, xT, False, f32(od_w_out)[0], f32(ffn_w_gate)[1], f32(ffn_w_up)[1], f32(ffn_w_down)[1], gains1)
    out = np.zeros((2, S, 1024), np.float32)
    for i in range(NCORE):
        out[i // 4, (i % 4) * TPC:(i % 4 + 1) * TPC, :] = xT[i].T
    return out


def layer_post(cfg, cst, resB, xT, even, w_out, w_gate, w_up, w_down, gains):
    TPC = cfg.TPC
    oT_l = []
    for i in range(NCORE):
        b, r = divmod(i, 4)
        cols = slice(r * TPC, (r + 1) * TPC)
        if even:
            fox = [resB[4 * b + hg]["oT"][0:128, cols] for hg in range(4)]
            nsa = [resB[4 * b + hg]["oT"][128:256, cols] for hg in range(4)]
            oT_l.append(np.ascontiguousarray(np.concatenate(fox + nsa, axis=0)))
        else:
            oT_l.append(np.ascontiguousarray(np.concatenate([resB[4 * b + hg]["oT"][:, cols] for hg in range(4)], axis=0)))
    resC = run_layer_C(cfg, cst, oT_l, xT, w_out, w_gate, w_up, w_down, gains)
    return [r["xT_out"] for r in resC]


def layer1(cfg, cst, xT, od_w_in, od_w_out, g_mix_pre, g_mix_post, g_ffn_pre, g_ffn_post, ffn_w_gate, ffn_w_up, ffn_w_down, debug=None):
    f32 = lambda a: np.ascontiguousarray(np.asarray(a, dtype=np.float32))
    S = cfg.S
    w = f32(od_w_in)[0]
    q, k, v = w[:, 0:1024], w[:, 1024:2048], w[:, 2048:3072]
    W1 = np.ascontiguousarray(np.concatenate([q, rot_cols(q), k, rot_cols(k), v], axis=1))
    gains1 = gains_layout(f32(g_mix_post)[1], f32(g_ffn_pre)[1], f32(g_ffn_post)[1], f32(g_mix_pre)[1])
    resA = run_layer_A(cfg, cst, False, xT, W1, gains1)
    if debug is not None:
        debug["A1"] = resA
    qT_l, kT_l, vp_l = [], [], []
    for i in range(NCORE):
        b, hg = divmod(i, 4)
        qT_l.append(cat_tok(resA, "qT", b, slice(hg * 256, (hg + 1) * 256)))
        kT_l.append(cat_tok(resA, "kT", b, slice(hg * 256, (hg + 1) * 256)))
        vpb = np.concatenate([resA[4 * b + r]["vp"][4 * hg:4 * hg + 4] for r in range(4)], axis=2)
        vp_l.append(np.ascontiguousarray(vpb))
    resB = run_B_moba(cfg, cst, qT_l, kT_l, vp_l)
    if debug is not None:
        debug["B1"] = resB
    return layer_post(cfg, cst, resB, xT, False, f32(od_w_out)[0], f32(ffn_w_gate)[1], f32(ffn_w_up)[1], f32(ffn_w_down)[1], gains1)


def kernel(**inputs):
    cfg = Cfg(int(np.asarray(inputs["x"]).shape[1]))
    return forward(cfg, **inputs)
```
